# Optimizing a Trainium2 kernel written in Bass

```python
import jax
import jax.numpy as jnp
from jax import lax
import numpy as np

D_MODEL = 1024
BATCH = 2
SEQ = 8192
DEPTH = 1

GRID_W = 64
CTX_LEN = 256
D_MIX = D_MODEL
CONV_CH = D_MIX // 2
CONV_WIDTH = 31
N_HEADS = 8
HEAD_DIM = (D_MIX - CONV_CH) // N_HEADS
ATTN_W = N_HEADS * HEAD_DIM
NA_ROWS = 8
NA_COLS = 16
QB_W = 16
KB_W = 32
N_EXPERTS = 16
D_EXPERT = 2048
EC_FACTOR = 2
N_MOD = 6
EPS = 1e-6
NEG_INF = -1e30
W_IN_COLS = 2 * CONV_CH + 3 * ATTN_W

kernel_name = 'hybrid_conformer_natten_ec_dit'


def rms_norm(x, g):
    xf = x.astype(jnp.float32)
    y = xf * lax.rsqrt(jnp.mean(xf * xf, axis=-1, keepdims=True) + EPS)
    return (y * g.astype(jnp.float32)).astype(x.dtype)


def layer_norm(x, g, b):
    xf = x.astype(jnp.float32)
    mu = jnp.mean(xf, axis=-1, keepdims=True)
    var = jnp.mean(jnp.square(xf - mu), axis=-1, keepdims=True)
    y = (xf - mu) * lax.rsqrt(var + EPS)
    return (y * g.astype(jnp.float32) + b.astype(jnp.float32)).astype(x.dtype)


def adaln(cvec, w_mod, b_mod):
    m = (jax.nn.silu(cvec) @ w_mod + b_mod).reshape(-1, 1, N_MOD * D_MODEL)
    return jnp.split(m, N_MOD, axis=-1)


def modulate(h, shift, scale):
    return h * (1.0 + scale) + shift


def conv_mixer(u, conv_w, conv_b, ln_g, ln_b):
    a, g = jnp.split(u, 2, axis=-1)
    v = a * jax.nn.sigmoid(g)
    y = lax.conv_general_dilated(
        v, conv_w[:, None, :].astype(v.dtype), window_strides=(1,),
        padding=[(CONV_WIDTH // 2, CONV_WIDTH // 2)],
        dimension_numbers=('NWC', 'WIO', 'NWC'), feature_group_count=CONV_CH) + conv_b
    return jax.nn.silu(layer_norm(y, ln_g, ln_b))


def na_indices(rows):
    kh = min(NA_ROWS, rows)
    n_blk = GRID_W // QB_W
    r = jnp.arange(rows)
    r0 = jnp.clip(r - kh // 2, 0, rows - kh)
    key_rows = r0[:, None] + jnp.arange(kh)[None, :]
    q_cols = jnp.arange(GRID_W).reshape(n_blk, QB_W)
    band0 = jnp.clip(jnp.arange(n_blk) * QB_W - (KB_W - QB_W) // 2, 0, GRID_W - KB_W)
    key_cols = band0[:, None] + jnp.arange(KB_W)[None, :]
    c0 = jnp.clip(q_cols - NA_COLS // 2, 0, GRID_W - NA_COLS)
    kc_b = key_cols[:, None, :]
    col_valid = (kc_b >= c0[:, :, None]) & (kc_b < c0[:, :, None] + NA_COLS)
    d_row = key_rows - r[:, None] + (NA_ROWS - 1)
    d_col = jnp.clip(kc_b - q_cols[:, :, None] + (NA_COLS - 1), 0, 2 * NA_COLS - 2)
    return key_rows, key_cols, col_valid, d_row, d_col


def na_attention(q, k, v, k_ctx, v_ctx, rpb, idx):
    key_rows, key_cols, col_valid, d_row, d_col = idx
    B, T, _ = q.shape
    rows, kh = key_rows.shape
    n_blk = GRID_W // QB_W
    L = k_ctx.shape[1]
    f32 = jnp.float32
    qb = q.reshape(B, rows, n_blk, QB_W, N_HEADS, HEAD_DIM).astype(f32) * (HEAD_DIM ** -0.5)
    kg = k.reshape(B, rows, GRID_W, N_HEADS, HEAD_DIM)
    vg = v.reshape(B, rows, GRID_W, N_HEADS, HEAD_DIM)
    ri = key_rows[:, None, :, None]
    ci = key_cols[None, :, None, :]
    k_band = kg[:, ri, ci]
    v_band = vg[:, ri, ci]
    kcx = k_ctx.reshape(B, L, N_HEADS, HEAD_DIM)
    vcx = v_ctx.reshape(B, L, N_HEADS, HEAD_DIM)
    bias = rpb[:, d_row[:, None, None, :, None], d_col[None, :, :, None, :]]
    s_loc = jnp.einsum('brnqhd,brnjwhd->bhrnqjw', qb, k_band.astype(f32)) + bias.astype(f32)[None]
    s_loc = jnp.where(col_valid[:, :, None, :], s_loc, NEG_INF)
    s_ctx = jnp.einsum('brnqhd,blhd->bhrnql', qb, kcx.astype(f32))
    n_loc = kh * KB_W
    s = jnp.concatenate([s_loc.reshape(s_loc.shape[:5] + (n_loc,)), s_ctx], axis=-1)
    p = jax.nn.softmax(s, axis=-1).astype(v.dtype)
    p_loc = p[..., :n_loc].reshape(s_loc.shape)
    p_ctx = p[..., n_loc:]
    o = (jnp.einsum('bhrnqjw,brnjwhd->brnqhd', p_loc, v_band)
         + jnp.einsum('bhrnql,blhd->brnqhd', p_ctx, vcx))
    return o.reshape(B, T, ATTN_W)


def ctx_attention(q, k, v):
    B, L, _ = q.shape
    f32 = jnp.float32
    qh = q.reshape(B, L, N_HEADS, HEAD_DIM).astype(f32) * (HEAD_DIM ** -0.5)
    kh = k.reshape(B, L, N_HEADS, HEAD_DIM).astype(f32)
    vh = v.reshape(B, L, N_HEADS, HEAD_DIM)
    p = jax.nn.softmax(jnp.einsum('blhd,bmhd->bhlm', qh, kh), axis=-1).astype(v.dtype)
    return jnp.einsum('bhlm,bmhd->blhd', p, vh).reshape(B, L, ATTN_W)


def ec_moe(h, w_router, w_gate, w_up, w_down):
    B, T, _ = h.shape
    cap = EC_FACTOR * T // N_EXPERTS
    aff = jax.nn.softmax((h @ w_router).astype(jnp.float32), axis=-1)
    wts, tok = lax.top_k(jnp.swapaxes(aff, 1, 2), cap)
    bidx = jnp.arange(B)[:, None, None]
    xs = h[bidx, tok]
    hid = jax.nn.silu(jnp.einsum('becd,edf->becf', xs, w_gate)) * jnp.einsum('becd,edf->becf', xs, w_up)
    y = jnp.einsum('becf,efd->becd', hid, w_down) * wts[..., None].astype(h.dtype)
    return jnp.zeros_like(h).at[bidx, tok].add(y)


def setup_inputs(seed: int = 0) -> dict:
    key = jax.random.key(seed)
    ks = jax.random.split(key, 21)
    f32 = jnp.float32

    def nrm(k, shape, scale):
        return jax.random.normal(k, shape, f32) * scale

    return {
        'x': nrm(ks[0], (BATCH, SEQ, D_MODEL), 1.0),
        'c': nrm(ks[1], (BATCH, D_MODEL), 1.0),
        'ctx': nrm(ks[2], (BATCH, CTX_LEN, D_MODEL), 1.0),
        'c_ctx': nrm(ks[3], (D_MODEL,), 1.0),
        'w_mod': nrm(ks[4], (DEPTH, D_MODEL, N_MOD * D_MODEL), 0.5 * D_MODEL ** -0.5),
        'b_mod': nrm(ks[5], (DEPTH, N_MOD * D_MODEL), 0.02),
        'norm_mix_g': 1.0 + nrm(ks[6], (DEPTH, D_MODEL), 0.02),
        'w_in': nrm(ks[7], (DEPTH, D_MODEL, W_IN_COLS), D_MODEL ** -0.5),
        'conv_w': nrm(ks[8], (DEPTH, CONV_WIDTH, CONV_CH), CONV_WIDTH ** -0.5),
        'conv_b': nrm(ks[9], (DEPTH, CONV_CH), 0.02),
        'conv_ln_g': 1.0 + nrm(ks[10], (DEPTH, CONV_CH), 0.02),
        'conv_ln_b': nrm(ks[11], (DEPTH, CONV_CH), 0.02),
        'rpb': nrm(ks[12], (DEPTH, N_HEADS, 2 * NA_ROWS - 1, 2 * NA_COLS - 1), 0.1),
        'w_out': nrm(ks[13], (DEPTH, D_MIX, D_MODEL), D_MIX ** -0.5),
        'norm_ffn_g': 1.0 + nrm(ks[14], (DEPTH, D_MODEL), 0.02),
        'w_router': nrm(ks[15], (DEPTH, D_MODEL, N_EXPERTS), D_MODEL ** -0.5),
        'w_gate': nrm(ks[16], (DEPTH, N_EXPERTS, D_MODEL, D_EXPERT), D_MODEL ** -0.5),
        'w_up': nrm(ks[17], (DEPTH, N_EXPERTS, D_MODEL, D_EXPERT), D_MODEL ** -0.5),
        'w_down': nrm(ks[18], (DEPTH, N_EXPERTS, D_EXPERT, D_MODEL), D_EXPERT ** -0.5),
        'norm_final_g': 1.0 + nrm(ks[19], (D_MODEL,), 0.02),
    }


def reference(x, c, ctx, c_ctx, w_mod, b_mod, norm_mix_g, w_in, conv_w, conv_b, conv_ln_g, conv_ln_b,
              rpb, w_out, norm_ffn_g, w_router, w_gate, w_up, w_down, norm_final_g):
    B, T, _ = x.shape
    rows = T // GRID_W
    idx = na_indices(rows)
    splits = [2 * CONV_CH, 2 * CONV_CH + ATTN_W, 2 * CONV_CH + 2 * ATTN_W]
    for l in range(DEPTH):
        last = l == DEPTH - 1
        sh1, sc1, g1, sh2, sc2, g2 = adaln(c, w_mod[l], b_mod[l])
        csh1, csc1, cg1, csh2, csc2, cg2 = adaln(c_ctx, w_mod[l], b_mod[l])

        h = modulate(rms_norm(x, norm_mix_g[l]), sh1, sc1)
        hc = modulate(rms_norm(ctx, norm_mix_g[l]), csh1, csc1)
        u, q, k, v = jnp.split(h @ w_in[l], splits, axis=-1)
        if last:
            k_c, v_c = jnp.split(hc @ w_in[l][:, splits[1]:], 2, axis=-1)
        else:
            u_c, q_c, k_c, v_c = jnp.split(hc @ w_in[l], splits, axis=-1)
        y_conv = conv_mixer(u, conv_w[l], conv_b[l], conv_ln_g[l], conv_ln_b[l])
        y_attn = na_attention(q, k, v, k_c, v_c, rpb[l], idx)
        x = x + g1 * (jnp.concatenate([y_conv, y_attn], axis=-1) @ w_out[l])
        if not last:
            yc_conv = conv_mixer(u_c, conv_w[l], conv_b[l], conv_ln_g[l], conv_ln_b[l])
            yc_attn = ctx_attention(q_c, k_c, v_c)
            ctx = ctx + cg1 * (jnp.concatenate([yc_conv, yc_attn], axis=-1) @ w_out[l])

        h2 = modulate(rms_norm(x, norm_ffn_g[l]), sh2, sc2)
        x = x + g2 * ec_moe(h2, w_router[l], w_gate[l], w_up[l], w_down[l])
        if not last:
            hc2 = modulate(rms_norm(ctx, norm_ffn_g[l]), csh2, csc2)
            ctx = ctx + cg2 * ec_moe(hc2, w_router[l], w_gate[l], w_up[l], w_down[l])
    return rms_norm(x, norm_final_g)
```

```python
import numpy as np
import concourse.bass as bass
import concourse.mybir as mybir
from concourse.bass_utils import run_bass_kernel_spmd

F32 = mybir.dt.float32
BF16 = mybir.dt.bfloat16
I32 = mybir.dt.int32
ALU = mybir.AluOpType
AF = mybir.ActivationFunctionType
AX = mybir.AxisListType

D = 1024
T = 8192
NCORE = 8
TOK = 2048
HALO = 2560
NEG = -30000.0
EPS = 1e-6
STAGE = 99
SAME_ENGINE_SYNC = True
NDMA = 24
NEXP = 16
NCH = 4
CUT = 0
SUB = 0
NITER = 26
USE_RELAY = False


class Prog:
    def __init__(self, nc):
        self.nc = nc
        self.q = {k: [] for k in ("pe", "act", "dve", "pool", "sp")}
        self.esem = {k: nc.alloc_semaphore("es_" + k) for k in ("pe", "act", "dve", "pool")}
        self.cnt = {k: 0 for k in self.esem}
        self.dsem = [nc.alloc_semaphore("ds%d" % i) for i in range(NDMA)]
        self.dcnt = [0] * NDMA
        self.rr = 0
        self.ccsem = nc.alloc_semaphore("ccsem")
        self.cccnt = 0
        self.lastw = {}
        self.readers = {}
        self.known = {k: {} for k in self.q}

    def _deps(self, reads, writes):
        deps = []
        for k in reads:
            if k in self.lastw:
                deps.append(self.lastw[k] + ("raw",))
        for k in writes:
            if k in self.lastw:
                deps.append(self.lastw[k] + ("waw",))
            deps.extend(t + ("war",) for t in self.readers.get(k, ()))
        return deps

    def _filter(self, eng, deps):
        out = []
        kn = self.known[eng]
        best = {}
        for (sem, val, src, kind) in deps:
            if src == eng and (USE_RELAY or eng == "pe" or not SAME_ENGINE_SYNC or kind != "raw"):
                continue
            key = id(sem)
            if kn.get(key, 0) >= val:
                continue
            if key not in best or best[key][1] < val:
                best[key] = (sem, val)
        for key, (sem, val) in best.items():
            kn[key] = val
            out.append((sem, val))
        return out

    def _update(self, reads, writes, tok):
        for k in writes:
            self.lastw[k] = tok
            self.readers[k] = []
        for k in reads:
            self.readers.setdefault(k, []).append(tok)

    def _relay(self, eng, deps):
        if eng == "pe" or not SAME_ENGINE_SYNC or not USE_RELAY:
            return deps
        out = []
        need = 0
        for d in deps:
            (sem, val, src, kind) = d
            if src == eng and kind == "raw":
                if self.cnt[eng] - val >= 16:
                    continue
                need = max(need, val)
            else:
                out.append(d)
        if need:
            r = "act" if eng == "pool" else "pool"
            dummy = self.dummy
            if r == "pool":
                fn = lambda e: e.memset(dummy[0:1, 0:1], 0.0)
            else:
                fn = lambda e: e.activation(out=dummy[0:1, 1:2], in_=dummy[0:1, 1:2], func=AF.Copy)
            self.cnt[r] += 1
            tok = (self.esem[r], self.cnt[r], r)
            self.q[r].append((fn, self._filter(r, [(self.esem[eng], need, eng, "x")]), (self.esem[r], 1)))
            out.append(tok + ("raw",))
        return out

    def op(self, eng, fn, reads=(), writes=()):
        deps = self._relay(eng, self._deps(reads, writes))
        self.cnt[eng] += 1
        tok = (self.esem[eng], self.cnt[eng], eng)
        self.q[eng].append((fn, self._filter(eng, deps), (self.esem[eng], 1)))
        self._update(reads, writes, tok)
        return tok

    def dma(self, queue, fn, reads=(), writes=()):
        deps = self._deps(reads, writes)
        k = self.rr
        self.rr = (k + 1) % NDMA
        sem = self.dsem[k]
        if self.dcnt[k] > 0:
            deps.append((sem, self.dcnt[k], "dma", "raw"))
        self.dcnt[k] += 16
        tok = (sem, self.dcnt[k], "dma")
        self.q[queue].append((fn, self._filter(queue, deps), (sem, 16)))
        self._update(reads, writes, tok)
        return tok

    def cc(self, fn, reads=(), writes=()):
        deps = self._deps(reads, writes)
        self.cccnt += 1
        tok = (self.ccsem, self.cccnt, "cc")
        self.q["pool"].append((fn, self._filter("pool", deps), (self.ccsem, 1)))
        self._update(reads, writes, tok)
        return tok

    def snapshot(self):
        import copy
        return dict(ql={k: len(v) for k, v in self.q.items()}, cnt=dict(self.cnt), dcnt=list(self.dcnt), rr=self.rr,
                    cccnt=self.cccnt, lastw=dict(self.lastw), readers={k: list(v) for k, v in self.readers.items()},
                    known={k: dict(v) for k, v in self.known.items()})

    def restore(self, sn):
        for k in self.q:
            del self.q[k][sn["ql"][k]:]
        self.cnt = dict(sn["cnt"]); self.dcnt = list(sn["dcnt"]); self.rr = sn["rr"]; self.cccnt = sn["cccnt"]
        self.lastw = dict(sn["lastw"]); self.readers = {k: list(v) for k, v in sn["readers"].items()}
        self.known = {k: dict(v) for k, v in sn["known"].items()}

    def barrier(self):
        toks = [(self.esem[k], self.cnt[k], k) for k in self.esem if self.cnt[k] > 0]
        toks += [(self.dsem[i], self.dcnt[i], "dma") for i in range(NDMA) if self.dcnt[i] > 0]
        if self.cccnt:
            toks.append((self.ccsem, self.cccnt, "cc"))
        for eng in self.q:
            deps = [t for t in toks if t[2] != eng or eng != "pe"]
            w = []
            kn = self.known[eng]
            for (sem, val, src) in deps:
                if kn.get(id(sem), 0) >= val:
                    continue
                kn[id(sem)] = val
                w.append((sem, val))
            if w:
                self.q[eng].append((None, w, None))
        self.lastw = {}
        self.readers = {}

    def emit(self):
        nc = self.nc
        q = self.q

        def replay(e, name):
            for (fn, waits, inc) in q[name]:
                for (sem, val) in waits:
                    e.wait_ge(sem, val)
                if fn is not None:
                    ins = fn(e)
                    ins.then_inc(inc[0], inc[1])

        with nc.Block() as block:
            @block.tensor
            def _(e):
                replay(e, "pe")

            @block.scalar
            def _(e):
                replay(e, "act")

            @block.vector
            def _(e):
                replay(e, "dve")

            @block.gpsimd
            def _(e):
                replay(e, "pool")

            @block.sync
            def _(e):
                replay(e, "sp")


def build_nc():
    nc = bass.Bass("TRN2", target_bir_lowering=False)
    P = Prog(nc)
    P.dummy = nc.alloc_sbuf_tensor("s_dummy", [1, 8], F32)

    TINY = {"w_mod": [128, 8], "w_in": [128, 8], "bias_tab": [16, 1, 8], "w_gate": [1, 128, 8],
            "w_up": [1, 128, 8], "w_down": [1, 128, 8]}

    def din(name, shape, dt=F32):
        if STAGE == 4 and name in TINY:
            shape = TINY[name]
        return nc.dram_tensor(name, list(shape), dt, kind="ExternalInput")

    xh = din("xh", [NCH, HALO, D])
    tokmask = din("tokmask", [NCH, 1, HALO])
    cvec = din("cvec", [2, D])
    ctx_d = din("ctx", [256, D])
    w_mod = din("w_mod", [D, 6 * D])
    b_mod = din("b_mod", [1, 6 * D])
    g_mix = din("norm_mix_g", [1, D])
    g_ffn = din("norm_ffn_g", [1, D])
    g_fin = din("norm_final_g", [1, D])
    w_in = din("w_in", [D, 2560])
    conv_w = din("conv_w", [31, 512])
    conv_b = din("conv_b", [1, 512])
    cln_g = din("conv_ln_g", [1, 512])
    cln_b = din("conv_ln_b", [1, 512])
    bias_tab = din("bias_tab", [5, 128, 8 * 6 * 128])
    bias_patch = din("bias_patch", [4, 128, 8 * 6 * 128])
    bsel_d = din("bsel", [128, NCH, 2])
    w_out = din("w_out", [D, D])
    w_router = din("w_router", [D, 16])
    w_gate = din("w_gate", [NEXP, D, 2048])
    w_up = din("w_up", [NEXP, D, 2048])
    w_down = din("w_down", [NEXP, 2048, D])
    ident_d = din("ident", [128, 128])
    out_d = nc.dram_tensor("out", [TOK, D], F32, kind="ExternalOutput")

    mod_d = nc.dram_tensor("mod_d", [2, 6 * D], F32)
    x1_d = nc.dram_tensor("x1_d", [TOK, D], F32)
    thr_d = nc.dram_tensor("thr_d", [1, 16], F32)
    aff_loc = nc.dram_tensor("aff_loc", [NCH, 16, TOK], F32)

    def sb(name, shape, dt):
        return nc.alloc_sbuf_tensor("s_" + name, list(shape), dt)

    def ps(name, shape, dt=F32):
        return nc.alloc_psum_tensor("p_" + name, list(shape), dt)

    ident = sb("ident", [128, 128], F32)
    ident_bf = sb("ident_bf", [128, 128], BF16)
    ones_bf = sb("ones_bf", [128, 128], BF16)
    maskb = sb("maskb", [128, 32], F32)
    cT = sb("cT", [128, 8, 2], F32)
    ones2 = sb("ones2", [1, 2], F32)
    G1 = sb("G1", [128, D], F32)
    A2 = sb("A2", [128, D], F32)
    B2 = sb("B2", [128, D], F32)
    G2 = G1
    GF = A2
    chv = sb("chv", [128, 8, 6], F32)
    A1T = sb("A1T", [128, 8], F32)
    cA1T = sb("cA1T", [128, 8], F32)
    cvp = sb("cvp", [128, 4, 3], F32)
    convwT = sb("convwT", [128, 4, 31], F32)
    small = sb("small", [128, 64], F32)
    aff_tm = sb("aff_tm", [128, 16, 16], F32)
    w_tm = sb("w_tm", [128, 16, 16], F32)
    wr = sb("wr", [128, 8, 16], F32)
    bis = sb("bis", [128, 8, 32], F32)
    cnt_bf = sb("cnt_bf", [128, 32], BF16)
    bcol = sb("bcol", [32, 4], F32)
    bselt = sb("bselt", [128, NCH, 4], F32)

    arenaA = sb("arenaA", [128, 40960], BF16)
    arenaB = sb("arenaB", [128, 41280], BF16)
    stg0 = sb("stg0", [128, 1536], F32)
    stg1 = sb("stg1", [128, 1536], F32)
    xs0 = sb("xs0", [128, D], F32)
    xs1 = sb("xs1", [128, D], F32)
    junk = sb("junk", [128, D], BF16)

    psb = [ps("psb%d" % i, [128, 512], F32) for i in range(7)]
    pstr = ps("pstr", [128, 1024], BF16)

    arenaA_f = arenaA[:, :].bitcast(F32)
    arenaB_f = arenaB[:, :].bitcast(F32)
    hT = arenaA[:, 0:20480].rearrange("p (c t) -> p c t", c=8)
    w_bf = arenaA[:, 20480:40960].rearrange("p (c t) -> p c t", c=8)
    o = 0
    vT = arenaB[:, o:o + 4 * 2080].rearrange("p (c t) -> p c t", c=4); o += 4 * 2080
    qT = arenaB[:, o:o + 4 * 2048].rearrange("p (c t) -> p c t", c=4); o += 4 * 2048
    kT = arenaB[:, o:o + 4 * 2560].rearrange("p (c t) -> p c t", c=4); o += 4 * 2560
    v_tm = arenaB[:, o:o + 20 * 520].rearrange("p (t h e) -> p t h e", t=20, h=8); o += 20 * 520
    kcT = arenaB[:, o:o + 4 * 256].rearrange("p (c t) -> p c t", c=4); o += 4 * 256
    vc_tm = arenaB[:, o:o + 2 * 520].rearrange("p (t h e) -> p t h e", t=2, h=8); o += 2 * 520
    hcT = arenaB[:, o:o + 8 * 256].rearrange("p (c t) -> p c t", c=8); o += 8 * 256
    assert o <= 41280, o

    sp = "sp"

    P.dma(sp, lambda e: e.dma_start(out=ident[:, :], in_=ident_d[:, :]), writes=["ident"])
    P.op("dve", lambda e: e.memset(ones2[:, :], 1.0), writes=["ones2"])
    P.dma(sp, lambda e: e.dma_start(out=bselt[:, :, 0:2], in_=bsel_d[:, :, :]), writes=["bsel"])
    P.op("dve", lambda e: e.tensor_scalar(out=bselt[:, :, 2:4], in0=bselt[:, :, 0:2], scalar1=-1.0, scalar2=1.0,
                                          op0=ALU.mult, op1=ALU.add), reads=["bsel"], writes=["bsel2"])
    P.op("dve", lambda e: e.memset(P.dummy[:, :], 0.0), writes=["dummy"])
    for r in range(2):
        P.dma(sp, (lambda e, r=r: e.dma_start(out=cT[:, :, r:r + 1], in_=cvec[r:r + 1, :].rearrange("r (c p) -> p c r", p=128),
                                              allow_slow_non_contiguous=True)), writes=["cT"])
    P.op("dve", lambda e: e.tensor_copy(out=ident_bf[:, :], in_=ident[:, :]), reads=["ident"], writes=["ident_bf"])
    P.op("dve", lambda e: e.memset(ones_bf[:, :], 1.0), writes=["ones_bf"])
    P.op("act", lambda e: e.activation(out=cT[:, :, :], in_=cT[:, :, :], func=AF.Silu), reads=["cT"], writes=["cT"])

    snap0 = P.snapshot()
    stgs = [stg0, stg1]
    mrow = stg0[0:2, 1024:1536]
    bmc = stg1[0:1, 1024:1536]
    wm_v = w_mod.ap().rearrange("(c p) n -> p c n", p=128)
    ld = 0
    for n in range(12):
        P.dma(sp, (lambda e, n=n: e.dma_start(out=bmc, in_=b_mod[0:1, n * 512:(n + 1) * 512])),
              writes=["bmc"])
        for q4 in range(4):
            st = stgs[ld % 2]
            key = "stg%d" % (ld % 2)
            ld += 1
            stv = st[:, 0:1024].rearrange("p (c n) -> p c n", c=2)
            P.dma(sp, (lambda e, stv=stv, n=n, q4=q4: e.dma_start(
                out=stv, in_=wm_v[:, q4 * 2:q4 * 2 + 2, n * 512:(n + 1) * 512])), writes=[key])
            for c2 in range(2):
                c = q4 * 2 + c2
                P.op("pe", (lambda e, stv=stv, c2=c2, c=c: e.matmul(
                    psb[0][0:2, :], lhsT=cT[:, c, :], rhs=stv[:, c2, :], start=(c == 0), stop=False)),
                    reads=[key, "cT"], writes=["ps0"])
        P.op("pe", (lambda e, n=n: e.matmul(psb[0][0:2, :], lhsT=ones2[:, :], rhs=bmc, start=False, stop=True)),
             reads=["bmc", "ones2"], writes=["ps0"])
        P.op("dve", (lambda e, n=n: e.tensor_copy(out=mrow, in_=psb[0][0:2, :])),
             reads=["ps0"], writes=["mrow"])
        P.dma(sp, (lambda e, n=n: e.dma_start(out=mod_d[:, n * 512:(n + 1) * 512], in_=mrow)),
              reads=["mrow"], writes=["mod_d"])

    def bc(dst, src_ap, key):
        P.dma(sp, lambda e: e.dma_start(out=dst[:, :], in_=src_ap.partition_broadcast(128)),
              reads=["mod_d"], writes=[key])

    bc(G1, mod_d[0:1, 2 * D:3 * D], "G1")
    bc(B2, mod_d[0:1, 3 * D:4 * D], "B2")
    bc(A2, mod_d[0:1, 4 * D:5 * D], "A2")
    bc(xs0, g_ffn[0:1, :], "xs0")
    P.op("dve", lambda e: e.scalar_tensor_tensor(out=A2[:, :], in0=A2[:, :], scalar=1.0, in1=xs0[:, :],
                                                 op0=ALU.add, op1=ALU.mult), reads=["A2", "xs0"], writes=["A2"])

    def chload(idx, src_ap):
        P.dma(sp, lambda e: e.dma_start(out=chv[:, :, idx:idx + 1], in_=src_ap.rearrange("r (c p) -> p c r", p=128),
                                        allow_slow_non_contiguous=True), reads=["mod_d"], writes=["chv%d" % idx])

    chload(0, mod_d[0:1, 0:D])
    chload(1, mod_d[0:1, D:2 * D])
    chload(2, mod_d[1:2, 0:D])
    chload(3, mod_d[1:2, D:2 * D])
    chload(4, g_mix[0:1, :])
    P.op("dve", lambda e: e.scalar_tensor_tensor(out=A1T[:, :], in0=chv[:, :, 1], scalar=1.0, in1=chv[:, :, 4],
                                                 op0=ALU.add, op1=ALU.mult), reads=["chv1", "chv4"], writes=["A1T"])
    P.op("dve", lambda e: e.scalar_tensor_tensor(out=cA1T[:, :], in0=chv[:, :, 3], scalar=1.0, in1=chv[:, :, 4],
                                                 op0=ALU.add, op1=ALU.mult), reads=["chv3", "chv4"], writes=["cA1T"])
    for i, src in enumerate((conv_b, cln_g, cln_b)):
        P.dma(sp, (lambda e, i=i, src=src: e.dma_start(out=cvp[:, :, i:i + 1],
                                                       in_=src.ap().rearrange("r (c p) -> p c r", p=128),
                                                       allow_slow_non_contiguous=True)), writes=["cvp%d" % i])
    for cc in range(4):
        P.dma(sp, (lambda e, cc=cc: e.dma_start(out=convwT[:, cc, :], in_=conv_w[:, cc * 128:(cc + 1) * 128].rearrange("j p -> p j"),
                                                allow_slow_non_contiguous=True)), writes=["convwT"])

    for ch in range(NCH):
        P.dma(sp, (lambda e, ch=ch: e.dma_start(out=maskb[:, 0:16], in_=tokmask[ch, 0:1, 240:256].partition_broadcast(128))), writes=["maskb"])
        P.dma(sp, (lambda e, ch=ch: e.dma_start(out=maskb[:, 16:32], in_=tokmask[ch, 0:1, 2304:2320].partition_broadcast(128))), writes=["maskb"])
        snapA = P.snapshot()
        xss = [xs0, xs1]

        def norm_tile(src_ap, xi, dstT, t0, AT, BT_idx, tag):
            xt = xss[xi]
            xk = "xs%d" % xi
            P.dma(sp, lambda e: e.dma_start(out=xt[:, :], in_=src_ap), writes=[xk])
            P.op("pool", lambda e: e.memset(small[:, xi:xi + 1], 0.0), writes=["ss%d" % xi])
            P.op("act", lambda e: e.activation(out=junk[:, :], in_=xt[:, :], func=AF.Square, accum_out=small[:, xi:xi + 1]),
                 reads=[xk, "ss%d" % xi], writes=["junk", "ss%d" % xi])
            P.op("act", lambda e: e.activation(out=small[:, xi:xi + 1], in_=small[:, xi:xi + 1], func=AF.Sqrt,
                                               scale=1.0 / D, bias=EPS), reads=["ss%d" % xi], writes=["ss%d" % xi])
            P.op("dve", lambda e: e.reciprocal(out=small[:, xi:xi + 1], in_=small[:, xi:xi + 1]),
                 reads=["ss%d" % xi], writes=["ss%d" % xi])
            P.op("dve", lambda e: e.tensor_scalar(out=xt[:, :], in0=xt[:, :], scalar1=small[:, xi:xi + 1], scalar2=None,
                                                  op0=ALU.mult), reads=[xk, "ss%d" % xi], writes=[xk])
            for half in range(2):
                pb = psb[1 + half]
                pk = "ps%d" % (1 + half)
                for c4 in range(4):
                    dc = half * 4 + c4
                    P.op("pe", (lambda e, pb=pb, c4=c4, dc=dc: e.transpose(out=pb[:, c4 * 128:(c4 + 1) * 128],
                                                                           in_=xt[:, dc * 128:(dc + 1) * 128],
                                                                           identity=ident[:, :])),
                         reads=[xk, "ident"], writes=[pk])
                for c4 in range(4):
                    dc = half * 4 + c4
                    P.op("act", (lambda e, pb=pb, c4=c4, dc=dc: e.activation(
                        out=dstT[:, dc, t0:t0 + 128], in_=pb[:, c4 * 128:(c4 + 1) * 128], func=AF.Identity,
                        scale=AT[:, dc:dc + 1], bias=chv[:, dc, BT_idx:BT_idx + 1])),
                        reads=[pk, "A1T", "cA1T", "chv0", "chv2"], writes=[(tag, t0 // 128)])

        for tt in range(20):
            norm_tile(xh[ch, tt * 128:(tt + 1) * 128, :], tt % 2, hT, tt * 128, A1T, 0, "hT")
        for tt in range(2):
            norm_tile(ctx_d[tt * 128:(tt + 1) * 128, :], tt % 2, hcT, tt * 128, cA1T, 2, "hcT")

        for dc in range(8):
            for hf in range(2):
                st = stgs[hf]
                key = "stg%d" % hf
                P.dma(sp, (lambda e, st=st, dc=dc, hf=hf: e.dma_start(out=st[:, 0:1280],
                                                                     in_=w_in[dc * 128:(dc + 1) * 128, hf * 1280:(hf + 1) * 1280])),
                      writes=[key])
                P.op("pool" if hf else "dve", (lambda e, st=st, dc=dc, hf=hf: e.tensor_copy(
                    out=w_bf[:, dc, hf * 1280:(hf + 1) * 1280], in_=st[:, 0:1280])), reads=[key], writes=[("w_bf", dc, hf)])
        wkeys = [("w_bf", dc, hf) for dc in range(8) for hf in range(2)]
        hkeys = lambda t0, n: [("hT", k) for k in range(t0 // 128, (t0 + n + 127) // 128)]

        def mm_chan(pb, pk, col0, t0, n, src=hT, skeys=None):
            for dc in range(8):
                P.op("pe", (lambda e, dc=dc: e.matmul(pb[:, 0:n], lhsT=w_bf[:, dc, col0:col0 + 128],
                                                       rhs=src[:, dc, t0:t0 + n], start=(dc == 0), stop=(dc == 7))),
                     reads=wkeys + (skeys if skeys is not None else hkeys(t0, n)), writes=[pk])

        ublocks = [(240, 512), (752, 512), (1264, 512), (1776, 512), (2288, 32)]
        it = 0
        for (t0, n) in ublocks:
            for cc in range(4):
                pa, pg = psb[3 + 2 * (it % 2)], psb[4 + 2 * (it % 2)]
                ka, kg = "ps%d" % (3 + 2 * (it % 2)), "ps%d" % (4 + 2 * (it % 2))
                st = stgs[it % 2]
                sk = "stg%d" % (it % 2)
                it += 1
                mm_chan(pa, ka, cc * 128, t0, n)
                mm_chan(pg, kg, 512 + cc * 128, t0, n)
                P.op("act", (lambda e, pg=pg, st=st, n=n: e.activation(out=st[:, 0:n], in_=pg[:, 0:n], func=AF.Sigmoid)),
                     reads=[kg], writes=[sk])
                P.op("dve", (lambda e, pa=pa, st=st, n=n, t0=t0, cc=cc: e.tensor_tensor(
                    out=vT[:, cc, t0 - 240:t0 - 240 + n], in0=pa[:, 0:n], in1=st[:, 0:n], op=ALU.mult)),
                    reads=[ka, sk], writes=[("vT", cc)])
        for cc in range(4):
            P.op("dve", (lambda e, cc=cc: e.tensor_tensor(out=vT[:, cc, 0:16], in0=vT[:, cc, 0:16], in1=maskb[:, 0:16], op=ALU.mult)),
                 reads=[("vT", cc), "maskb"], writes=[("vT", cc)])
            P.op("dve", (lambda e, cc=cc: e.tensor_tensor(out=vT[:, cc, 2064:2080], in0=vT[:, cc, 2064:2080], in1=maskb[:, 16:32],
                                                          op=ALU.mult)), reads=[("vT", cc), "maskb"], writes=[("vT", cc)])
        for tb in range(4):
            for cc in range(4):
                pb, pk = psb[3 + (it % 4)], "ps%d" % (3 + (it % 4))
                it += 1
                mm_chan(pb, pk, 1024 + cc * 128, 256 + tb * 512, 512)
                P.op("act", (lambda e, pb=pb, cc=cc, tb=tb: e.activation(out=qT[:, cc, tb * 512:(tb + 1) * 512], in_=pb[:, :],
                                                                         func=AF.Copy, scale=0.125)),
                     reads=[pk], writes=[("qT", cc)])
        for tb in range(5):
            for cc in range(4):
                pb, pk = psb[3 + (it % 4)], "ps%d" % (3 + (it % 4))
                it += 1
                mm_chan(pb, pk, 1536 + cc * 128, tb * 512, 512)
                P.op("dve", (lambda e, pb=pb, cc=cc, tb=tb: e.tensor_copy(out=kT[:, cc, tb * 512:(tb + 1) * 512], in_=pb[:, :])),
                     reads=[pk], writes=[("kT", cc)])
        for cc in range(4):
            pb, pk = psb[3 + (it % 4)], "ps%d" % (3 + (it % 4))
            it += 1
            mm_chan(pb, pk, 1536 + cc * 128, 0, 256, src=hcT, skeys=[("hcT", 0), ("hcT", 1)])
            P.op("dve", (lambda e, pb=pb, cc=cc: e.tensor_copy(out=kcT[:, cc, :], in_=pb[:, 0:256])),
                 reads=[pk], writes=[("kcT", cc)])
        P.op("pool", lambda e: e.memset(v_tm[:, :, :, 64:65], 1.0), writes=["v_ones"])
        P.op("pool", lambda e: e.memset(vc_tm[:, :, :, 64:65], 1.0), writes=["vc_ones"])

        def mm_v(pb, pk, src, t0, skeys):
            for dc in range(8):
                P.op("pe", (lambda e, dc=dc: e.matmul(pb[:, :], lhsT=src[:, dc, t0:t0 + 128], rhs=w_bf[:, dc, 2048:2560],
                                                       start=(dc == 0), stop=(dc == 7))),
                     reads=wkeys + skeys, writes=[pk])

        for tt in range(20):
            pb, pk = psb[3 + (it % 4)], "ps%d" % (3 + (it % 4))
            it += 1
            mm_v(pb, pk, hT, tt * 128, [("hT", tt)])
            P.op("act" if tt % 2 else "dve", (lambda e, pb=pb, tt=tt: (e.activation(
                out=v_tm[:, tt, :, 0:64], in_=pb[:, :].rearrange("p (h e) -> p h e", h=8), func=AF.Copy)
                if tt % 2 else e.tensor_copy(out=v_tm[:, tt, :, 0:64], in_=pb[:, :].rearrange("p (h e) -> p h e", h=8)))),
                reads=[pk], writes=[("v_tm", tt)])
        for tt in range(2):
            pb, pk = psb[3 + (it % 4)], "ps%d" % (3 + (it % 4))
            it += 1
            mm_v(pb, pk, hcT, tt * 128, [("hcT", tt)])
            P.op("dve", (lambda e, pb=pb, tt=tt: e.tensor_copy(out=vc_tm[:, tt, :, 0:64],
                                                               in_=pb[:, :].rearrange("p (h e) -> p h e", h=8))),
                 reads=[pk], writes=[("vc_tm", tt)])

        P.barrier()

        diag = arenaA[:, 0:124 * 128].rearrange("p (j m) -> p j m", m=128)
        yc = arenaA[:, 15872:15872 + 2048].rearrange("p (c t) -> p c t", c=4)
        ysq = arenaA[:, 17920:17920 + 2048].rearrange("p (c t) -> p c t", c=4)
        yT = arenaA[:, 24576:40960].rearrange("p (c t) -> p c t", c=8)
        for j in range(31):
            for cc in range(4):
                P.op("dve" if (j + cc) % 2 else "pool", (lambda e, j=j, cc=cc: e.tensor_scalar(
                    out=diag[:, j * 4 + cc, :], in0=ident[:, :], scalar1=convwT[:, cc, j:j + 1], scalar2=None, op0=ALU.mult)),
                    reads=["ident", "convwT"], writes=[("diag", j, cc)])
        it = 0
        for tb in range(4):
            for cc in range(4):
                pb, pk = psb[it % 4], "ps%d" % (it % 4)
                it += 1
                for j in range(31):
                    P.op("pe", (lambda e, pb=pb, j=j, cc=cc, tb=tb: e.matmul(
                        pb[:, :], lhsT=diag[:, j * 4 + cc, :], rhs=vT[:, cc, tb * 512 + j + 1:tb * 512 + j + 513],
                        start=(j == 0), stop=(j == 30))), reads=[("diag", j, cc), ("vT", cc)], writes=[pk])
                P.op("act", (lambda e, pb=pb, cc=cc, tb=tb: e.activation(out=yc[:, cc, :], in_=pb[:, :],
                                                                         func=AF.Identity, bias=cvp[:, cc, 0:1])),
                     reads=[pk, "cvp0"], writes=[("yc", cc)])
                P.op("act", (lambda e, pb=pb, cc=cc, tb=tb: e.activation(out=ysq[:, cc, :], in_=pb[:, :],
                                                                         func=AF.Square, bias=cvp[:, cc, 0:1])),
                     reads=[pk, "cvp0"], writes=[("ysq", cc)])
            for cc in range(4):
                P.op("pe", (lambda e, cc=cc, tb=tb: e.matmul(psb[4][:, :], lhsT=ones_bf[:, :], rhs=yc[:, cc, :],
                                                              start=(cc == 0), stop=(cc == 3))),
                     reads=["ones_bf", ("yc", cc)], writes=["ps4"])
            for cc in range(4):
                P.op("pe", (lambda e, cc=cc, tb=tb: e.matmul(psb[5][:, :], lhsT=ones_bf[:, :], rhs=ysq[:, cc, :],
                                                              start=(cc == 0), stop=(cc == 3))),
                     reads=["ones_bf", ("ysq", cc)], writes=["ps5"])
            mean = stg0[:, 0:512]
            rstd = stg0[:, 512:1024]
            tmp = stg0[:, 1024:1536]
            P.op("dve", lambda e: e.tensor_scalar(out=mean, in0=psb[4][:, :], scalar1=1.0 / 512, scalar2=None, op0=ALU.mult),
                 reads=["ps4"], writes=["mean"])
            P.op("dve", lambda e: e.tensor_tensor(out=tmp, in0=mean, in1=mean, op=ALU.mult), reads=["mean"], writes=["tmp"])
            P.op("dve", lambda e: e.scalar_tensor_tensor(out=rstd, in0=psb[5][:, :], scalar=1.0 / 512, in1=tmp,
                                                         op0=ALU.mult, op1=ALU.subtract), reads=["ps5", "tmp"], writes=["rstd"])
            P.op("act", lambda e: e.activation(out=rstd, in_=rstd, func=AF.Sqrt, bias=EPS), reads=["rstd"], writes=["rstd"])
            P.op("dve", lambda e: e.reciprocal(out=rstd, in_=rstd), reads=["rstd"], writes=["rstd"])
            for cc in range(4):
                t1 = stg1[:, (cc % 2) * 512:(cc % 2) * 512 + 512]
                tk = "t1_%d" % (cc % 2)
                P.op("dve", (lambda e, cc=cc, tb=tb, t1=t1: e.tensor_tensor(out=t1, in0=yc[:, cc, :],
                                                                             in1=mean, op=ALU.subtract)),
                     reads=[("yc", cc), "mean"], writes=[tk])
                P.op("dve", (lambda e, t1=t1: e.tensor_tensor(out=t1, in0=t1, in1=rstd, op=ALU.mult)),
                     reads=[tk, "rstd"], writes=[tk])
                P.op("act", (lambda e, cc=cc, tb=tb, t1=t1: e.activation(out=yT[:, cc, tb * 512:(tb + 1) * 512], in_=t1,
                                                                         func=AF.Silu, scale=cvp[:, cc, 1:2], bias=cvp[:, cc, 2:3])),
                     reads=[tk, "cvp1", "cvp2"], writes=[("yT", cc, tb)])
        P.barrier()

        bstg = arenaA_f[:, 0:6144]
        EBs = [arenaA[:, 12288:18432], arenaA[:, 18432:24576]]
        stg1_bf = stg1[:, :].bitcast(BF16)
        pts = [stg1_bf[:, 0:1024], stg1_bf[:, 1024:2048]]
        yatts = [stg1_bf[:, 2048:2560], stg1_bf[:, 2560:3072]]
        rec = small[:, 8:24]
        ps_s = [psb[0], psb[1], psb[2], psb[3]]
        def emit_S(g, h):
            lo = min(max(2 * g - 2, 0), 28)
            cc, pofs = h // 2, (h % 2) * 64
            sA, sB = (psb[0], psb[1]) if h % 2 == 0 else (psb[2], psb[3])
            kA, kB = ("ps0", "ps1") if h % 2 == 0 else ("ps2", "ps3")
            Q = qT[pofs:pofs + 64, cc, g * 128:(g + 1) * 128]
            for jt in range(6):
                dst = sA[:, jt * 128:(jt + 1) * 128] if jt < 4 else sB[:, (jt - 4) * 128:(jt - 3) * 128]
                t0 = 64 * lo + jt * 128
                P.op("pe", (lambda e, dst=dst, t0=t0, cc=cc, pofs=pofs, Q=Q: e.matmul(
                    dst, lhsT=kT[pofs:pofs + 64, cc, t0:t0 + 128], rhs=Q, start=True, stop=True)),
                    reads=[("kT", cc), ("qT", cc)], writes=[kA if jt < 4 else kB])
            for jc in range(2):
                dst = sB[:, (2 + jc) * 128:(3 + jc) * 128]
                P.op("pe", (lambda e, dst=dst, jc=jc, cc=cc, pofs=pofs, Q=Q: e.matmul(
                    dst, lhsT=kcT[pofs:pofs + 64, cc, jc * 128:(jc + 1) * 128], rhs=Q, start=True, stop=True)),
                    reads=[("kcT", cc), ("qT", cc)], writes=[kB])

        emit_S(0, 0)
        for g in range(16):
            lo = min(max(2 * g - 2, 0), 28)
            EB = EBs[g % 2]
            ek = "EB%d" % (g % 2)
            gi = {0: 0, 1: 1, 14: 2, 15: 3}.get(g, 4)
            P.dma(sp, (lambda e, gi=gi: e.dma_start(out=bstg, in_=bias_tab[gi, :, :])), writes=["bstg"])
            if gi < 4:
                si = 0 if gi < 2 else 1
                for hf in range(2):
                    tmpH = arenaB_f[:, 0:3072]
                    P.dma(sp, (lambda e, gi=gi, hf=hf: e.dma_start(out=tmpH, in_=bias_patch[gi, :, hf * 3072:(hf + 1) * 3072])),
                          writes=["tmpH"])
                    P.op("dve", (lambda e, hf=hf, si=si, ch=ch: e.tensor_scalar(
                        out=bstg[:, hf * 3072:(hf + 1) * 3072], in0=bstg[:, hf * 3072:(hf + 1) * 3072],
                        scalar1=bselt[:, ch, 2 + si:3 + si], scalar2=None, op0=ALU.mult)), reads=["bstg", "bsel2"], writes=["bstg"])
                    P.op("dve", (lambda e, hf=hf, si=si, ch=ch: e.scalar_tensor_tensor(
                        out=bstg[:, hf * 3072:(hf + 1) * 3072], in0=tmpH, scalar=bselt[:, ch, si:si + 1],
                        in1=bstg[:, hf * 3072:(hf + 1) * 3072], op0=ALU.mult, op1=ALU.add)),
                        reads=["bstg", "tmpH", "bsel"], writes=["bstg"])
            for q4 in range(4):
                P.op("act", (lambda e, EB=EB, q4=q4: e.activation(out=EB[:, q4 * 1536:(q4 + 1) * 1536],
                                                                  in_=bstg[:, q4 * 1536:(q4 + 1) * 1536], func=AF.Exp)),
                     reads=["bstg"], writes=[(ek, q4)])
            po_t = [psb[4], psb[5]]
            for h in range(8):
                cc, pofs = h // 2, (h % 2) * 64
                sA, sB = (psb[0], psb[1]) if h % 2 == 0 else (psb[2], psb[3])
                kA, kB = ("ps0", "ps1") if h % 2 == 0 else ("ps2", "ps3")
                pt = pts[h % 2]
                ptk = "pt%d" % (h % 2)
                Q = qT[pofs:pofs + 64, cc, g * 128:(g + 1) * 128]
                nxt = (g, h + 1) if h < 7 else ((g + 1, 0) if g < 15 else None)
                if nxt is not None:
                    emit_S(*nxt)
                P.op("act", (lambda e, sA=sA, pt=pt: e.activation(out=pt[:, 0:512], in_=sA[:, :], func=AF.Exp)),
                     reads=[kA], writes=[ptk + "a"])
                P.op("act", (lambda e, sB=sB, pt=pt: e.activation(out=pt[:, 512:1024], in_=sB[:, :], func=AF.Exp)),
                     reads=[kB], writes=[ptk + "b"])
                P.op("dve", (lambda e, pt=pt, EB=EB, h=h: e.tensor_tensor(out=pt[:, 0:512], in0=pt[:, 0:512],
                                                                           in1=EB[:, h * 768:h * 768 + 512], op=ALU.mult)),
                     reads=[ptk + "a", (ek, (h * 768) // 1536), (ek, (h * 768 + 511) // 1536)], writes=[ptk + "a"])
                P.op("dve", (lambda e, pt=pt, EB=EB, h=h: e.tensor_tensor(out=pt[:, 512:768], in0=pt[:, 512:768],
                                                                           in1=EB[:, h * 768 + 512:h * 768 + 768], op=ALU.mult)),
                     reads=[ptk + "b", (ek, (h * 768 + 512) // 1536), (ek, (h * 768 + 767) // 1536)], writes=[ptk + "b"])
                po = po_t[h // 4]
                pok = "ps%d" % (4 + h // 4)
                hh = h % 4
                for jt in range(8):
                    if jt < 6:
                        rhs = v_tm[:, lo // 2 + jt, h, :]
                        rk = ("v_tm", lo // 2 + jt)
                    else:
                        rhs = vc_tm[:, jt - 6, h, :]
                        rk = ("vc_tm", jt - 6)
                    P.op("pe", (lambda e, po=po, hh=hh, jt=jt, pt=pt, rhs=rhs: e.matmul(
                        po[:, hh * 65:(hh + 1) * 65], lhsT=pt[:, jt * 128:(jt + 1) * 128], rhs=rhs,
                        start=(jt == 0), stop=(jt == 7))),
                        reads=[ptk + ("a" if jt < 4 else "b"), rk, "v_ones", "vc_ones"], writes=[pok])
            yatt = yatts[g % 2]
            yk = "yatt%d" % (g % 2)
            for half in range(2):
                po = po_t[half]
                pok = "ps%d" % (4 + half)
                pov = po[:, 0:260].rearrange("p (h e) -> p h e", h=4)
                P.op("dve", (lambda e, pov=pov, half=half: e.reciprocal(out=rec[:, half * 4:half * 4 + 4], in_=pov[:, :, 64])),
                     reads=[pok], writes=["rec%d" % half])
                for hh in range(4):
                    h = half * 4 + hh
                    if hh % 2 == 0:
                        P.op("act", (lambda e, pov=pov, hh=hh, h=h, yatt=yatt: e.activation(
                            out=yatt[:, h * 64:(h + 1) * 64], in_=pov[:, hh, 0:64], func=AF.Copy, scale=rec[:, h:h + 1])),
                            reads=[pok, "rec%d" % half], writes=[(yk, h)])
                    else:
                        P.op("dve", (lambda e, pov=pov, hh=hh, h=h, yatt=yatt: e.tensor_scalar(
                            out=yatt[:, h * 64:(h + 1) * 64], in0=pov[:, hh, 0:64], scalar1=rec[:, h:h + 1], scalar2=None,
                            op0=ALU.mult)), reads=[pok, "rec%d" % half], writes=[(yk, h)])
            ptrk = "pstr%d" % (g % 2)
            ptr_bf = pstr[:, (g % 2) * 512:(g % 2) * 512 + 512]
            for cc in range(4):
                P.op("pe", (lambda e, cc=cc, yatt=yatt, ptr_bf=ptr_bf: e.transpose(
                    out=ptr_bf[:, cc * 128:(cc + 1) * 128], in_=yatt[:, cc * 128:(cc + 1) * 128], identity=ident_bf[:, :])),
                    reads=[(yk, 2 * cc), (yk, 2 * cc + 1), "ident_bf"], writes=[ptrk])
            P.op("act" if g % 2 else "dve", (lambda e, g=g, ptr_bf=ptr_bf: (e.activation(
                out=yT[:, 4:8, g * 128:(g + 1) * 128], in_=ptr_bf[:, 0:512].rearrange("p (c t) -> p c t", c=4), func=AF.Copy)
                if g % 2 else e.tensor_copy(out=yT[:, 4:8, g * 128:(g + 1) * 128],
                                            in_=ptr_bf[:, 0:512].rearrange("p (c t) -> p c t", c=4)))),
                reads=[ptrk], writes=[("yTa", g)])
        P.barrier()

        if STAGE == 4:
            P.restore(snap0)
            P.op("dve", lambda e: e.memset(G1[:, :], 0.5), writes=["G1"])
            P.op("dve", lambda e: e.memset(A2[:, :], 1.0), writes=["A2"])
            P.op("dve", lambda e: e.memset(B2[:, :], 0.1), writes=["B2"])
            P.op("dve", lambda e: e.memset(arenaA[:, 24576:40960], 0.0), writes=["yTz"])
            P.barrier()
        if STAGE == 3:
            P.restore(snapA)
            P.op("dve", lambda e: e.memset(arenaA[:, 24576:40960], 0.0), writes=["yTz"])
            P.barrier()
        wo_bf = arenaA[:, 0:8192].rearrange("p (c n) -> p c n", c=8)
        for dc in range(8):
            st = stgs[dc % 2]
            key = "stg%d" % (dc % 2)
            P.dma(sp, (lambda e, st=st, dc=dc: e.dma_start(out=st[:, 0:1024], in_=w_out[dc * 128:(dc + 1) * 128, :])), writes=[key])
            P.op("pool" if dc % 2 else "dve", (lambda e, st=st, dc=dc: e.tensor_copy(out=wo_bf[:, dc, :], in_=st[:, 0:1024])),
                 reads=[key], writes=[("wo_bf", dc)])
        wokeys = [("wo_bf", dc) for dc in range(8)]
        h2T = arenaB[:, 0:16384].rearrange("p (c t) -> p c t", c=8)
        acc = arenaB_f[:, 8192:16384].rearrange("p (t d) -> p t d", t=8)
        stgG = arenaB_f[:, 16384:18432].rearrange("p (c f) -> p c f", c=8)
        stgU = arenaB_f[:, 18432:20480].rearrange("p (c f) -> p c f", c=8)
        h2f = stg1[:, 0:1024]
        h2hi = arenaA[:, 8192:9216]
        h2lo = arenaA[:, 9216:10240]
        wr_hi = arenaA[:, 10240:10368].rearrange("p (c e) -> p c e", c=8)
        wr_lo = arenaA[:, 10368:10496].rearrange("p (c e) -> p c e", c=8)
        h2Tlo = stg0[:, :].bitcast(BF16)[:, 2048:3072]
        hidT = arenaA[:, 0:16384].rearrange("p (c t) -> p c t", c=16)
        wd_bf = arenaA[:, 16384:32768].rearrange("p (c n) -> p c n", c=16)
        wg_bf = arenaA[:, 32768:36864].rearrange("p (b c f) -> p b c f", b=2, c=8)
        wu_bf = arenaA[:, 36864:40960].rearrange("p (b c f) -> p b c f", b=2, c=8)
        affE2 = arenaA_f[0:16, 0:8192]
        cmp2 = arenaA_f[0:16, 8192:16384]
        affE = arenaA_f[:, 0:2048].rearrange("p (q f) -> p q f", q=32)
        cmpb = arenaA_f[:, 2048:4096].rearrange("p (q f) -> p q f", q=32)
        affN = arenaA_f[:, 4096:6144].rearrange("p (b n) -> p b n", b=2)
        sgs = [stg0[:, 1024:1536], stg1[:, 1024:1536]]
        P.dma(sp, lambda e: e.dma_start(out=wr[:, :, :], in_=w_router.ap().rearrange("(c p) e -> p c e", p=128)), writes=["wr"])
        P.op("act", lambda e: e.activation(out=wr_hi, in_=wr[:, :, :], func=AF.Copy), reads=["wr"], writes=["wr_hi"])
        P.op("dve", lambda e: e.tensor_tensor(out=wr_lo, in0=wr[:, :, :], in1=wr_hi, op=ALU.subtract), reads=["wr", "wr_hi"], writes=["wr_lo"])
        for t in range(16):
            xt = xss[t % 2]
            xk = "xs%d" % (t % 2)
            P.dma(sp, (lambda e, xt=xt, t=t: e.dma_start(out=xt[:, :], in_=xh[ch, 256 + t * 128:256 + (t + 1) * 128, :])), writes=[xk])
            for half in range(2):
                pb = psb[(2 * t + half) % 4]
                pk = "ps%d" % ((2 * t + half) % 4)
                for kc in range(8):
                    P.op("pe", (lambda e, pb=pb, kc=kc, t=t, half=half: e.matmul(
                        pb[:, :], lhsT=yT[:, kc, t * 128:(t + 1) * 128], rhs=wo_bf[:, kc, half * 512:(half + 1) * 512],
                        start=(kc == 0), stop=(kc == 7))), reads=wokeys, writes=[pk])
                tmpf = stg0[:, half * 512:(half + 1) * 512]
                tk = "tmpf%d" % half
                P.op("dve", (lambda e, pb=pb, half=half, tmpf=tmpf: e.tensor_tensor(
                    out=tmpf, in0=pb[:, :], in1=G1[:, half * 512:(half + 1) * 512], op=ALU.mult)),
                    reads=[pk, "G1"], writes=[tk])
                P.op("pool", (lambda e, xt=xt, half=half, tmpf=tmpf: e.tensor_tensor(
                    out=xt[:, half * 512:(half + 1) * 512], in0=xt[:, half * 512:(half + 1) * 512], in1=tmpf, op=ALU.add)),
                    reads=[xk, tk], writes=[xk])
            P.dma(sp, (lambda e, xt=xt, t=t: e.dma_start(out=x1_d[t * 128:(t + 1) * 128, :], in_=xt[:, :])),
                  reads=[xk], writes=[("x1_d", t)])
            if STAGE == 1:
                P.dma(sp, (lambda e, xt=xt, t=t: e.dma_start(out=out_d[t * 128:(t + 1) * 128, :], in_=xt[:, :])),
                      reads=[xk], writes=[("out", t)])
                continue
            si = 2 + (t % 2)
            ssk = "ss%d" % si
            P.op("pool", (lambda e, si=si: e.memset(small[:, si:si + 1], 0.0)), writes=[ssk])
            P.op("act", (lambda e, xt=xt, si=si: e.activation(out=junk[:, :], in_=xt[:, :], func=AF.Square,
                                                              accum_out=small[:, si:si + 1])),
                 reads=[xk, ssk], writes=["junk", ssk])
            P.op("act", (lambda e, si=si: e.activation(out=small[:, si:si + 1], in_=small[:, si:si + 1], func=AF.Sqrt,
                                                       scale=1.0 / D, bias=EPS)), reads=[ssk], writes=[ssk])
            P.op("dve", (lambda e, si=si: e.reciprocal(out=small[:, si:si + 1], in_=small[:, si:si + 1])),
                 reads=[ssk], writes=[ssk])
            P.op("dve", (lambda e, xt=xt, si=si: e.scalar_tensor_tensor(out=h2f, in0=xt[:, :], scalar=small[:, si:si + 1],
                                                                        in1=A2[:, :], op0=ALU.mult, op1=ALU.mult)),
                 reads=[xk, ssk, "A2"], writes=["h2f"])
            P.op("pool", lambda e: e.tensor_tensor(out=h2f, in0=h2f, in1=B2[:, :], op=ALU.add), reads=["h2f", "B2"], writes=["h2f"])
            if SUB == 1:
                continue
            P.op("act", lambda e: e.activation(out=h2hi, in_=h2f, func=AF.Copy), reads=["h2f"], writes=["h2hi"])
            P.op("dve", lambda e: e.tensor_tensor(out=h2lo, in0=h2f, in1=h2hi, op=ALU.subtract), reads=["h2f", "h2hi"], writes=["h2lo"])
            for dc in range(8):
                P.op("pe", (lambda e, dc=dc: e.transpose(out=pstr[:, dc * 128:(dc + 1) * 128], in_=h2hi[:, dc * 128:(dc + 1) * 128],
                                                         identity=ident_bf[:, :])), reads=["h2hi", "ident_bf"], writes=["pstr"])
            P.op("act", (lambda e, t=t: e.activation(out=h2T[:, :, t * 128:(t + 1) * 128],
                                                     in_=pstr[:, :].rearrange("p (c t) -> p c t", c=8), func=AF.Copy)),
                 reads=["pstr"], writes=[("h2T", t)])
            for dc in range(8):
                P.op("pe", (lambda e, dc=dc: e.transpose(out=pstr[:, dc * 128:(dc + 1) * 128], in_=h2lo[:, dc * 128:(dc + 1) * 128],
                                                         identity=ident_bf[:, :])), reads=["h2lo", "ident_bf"], writes=["pstr"])
            P.op("dve", lambda e: e.tensor_copy(out=h2Tlo, in_=pstr[:, :]), reads=["pstr"], writes=["h2Tlo"])
            if SUB in (2, 5, 6, 7):
                continue
            k = 0
            for dc in range(8):
                for (lh, rh, lk) in ((h2T[:, dc, t * 128:(t + 1) * 128], wr_hi[:, dc, :], ("h2T", t)),
                                     (h2Tlo[:, dc * 128:(dc + 1) * 128], wr_hi[:, dc, :], "h2Tlo"),
                                     (h2T[:, dc, t * 128:(t + 1) * 128], wr_lo[:, dc, :], ("h2T", t))):
                    P.op("pe", (lambda e, lh=lh, rh=rh, k=k: e.matmul(psb[6][:, 0:16], lhsT=lh, rhs=rh, start=(k == 0), stop=(k == 23))),
                         reads=[lk, "wr_hi", "wr_lo"], writes=["ps6"])
                    k += 1
            if SUB == 3:
                continue
            P.op("dve", lambda e: e.reduce_max(out=small[:, 4:5], in_=psb[6][:, 0:16], axis=AX.X), reads=["ps6"], writes=["mx"])
            P.op("dve", lambda e: e.tensor_scalar(out=small[:, 4:5], in0=small[:, 4:5], scalar1=-1.0, scalar2=None, op0=ALU.mult),
                 reads=["mx"], writes=["mx"])
            P.op("pool", lambda e: e.memset(small[:, 5:6], 0.0), writes=["sm"])
            P.op("act", (lambda e, t=t: e.activation(out=aff_tm[:, t, :], in_=psb[6][:, 0:16], func=AF.Exp, bias=small[:, 4:5],
                                                     accum_out=small[:, 5:6])), reads=["ps6", "mx", "sm"], writes=[("aff", t), "sm"])
            P.op("dve", lambda e: e.reciprocal(out=small[:, 5:6], in_=small[:, 5:6]), reads=["sm"], writes=["sm"])
            P.op("dve", (lambda e, t=t: e.tensor_scalar(out=aff_tm[:, t, :], in0=aff_tm[:, t, :], scalar1=small[:, 5:6], scalar2=None,
                                                        op0=ALU.mult)), reads=[("aff", t), "sm"], writes=[("aff", t)])
        if STAGE != 1:
            for t in range(16):
                P.dma(sp, (lambda e, t=t, ch=ch: e.dma_start(out=aff_loc[ch, :, t * 128:(t + 1) * 128].rearrange("e p -> p e"),
                                                             in_=aff_tm[:, t, :], allow_slow_non_contiguous=True)),
                      reads=[("aff", t)], writes=["aff_loc"])
        P.barrier()
    def dbg_out():
        for t in range(16):
            P.dma(sp, (lambda e, t=t: e.dma_start(out=out_d[t * 128:(t + 1) * 128, 0:16], in_=w_tm[:, t, :])),
                  writes=[("out", t)])
            P.dma(sp, (lambda e, t=t: e.dma_start(out=out_d[t * 128:(t + 1) * 128, 16:32], in_=aff_tm[:, t, :])),
                  writes=[("out2", t)])
        P.dma(sp, lambda e: e.dma_start(out=out_d[0:128, 32:64], in_=bis[:, 0, :]), writes=["out3"])
        P.barrier()
        P.emit()

    if STAGE != 1:
        for c4 in range(NCH):
            P.dma(sp, (lambda e, c4=c4: e.dma_start(out=affE2[0:16, c4 * 2048:(c4 + 1) * 2048], in_=aff_loc[c4, :, :])),
                  reads=["aff_loc"], writes=["affE2"])
        bc(G2, mod_d[0:1, 5 * D:6 * D], "G2")
        bc(GF, g_fin[0:1, :], "GF")
        midc, gec, cntc, loc = bcol[0:16, 0:1], bcol[0:16, 1:2], bcol[0:16, 2:3], bcol[0:16, 3:4]
        P.op("dve", lambda e: e.memset(bcol[:, :], 0.5), writes=["midc"])
        for itn in range(NITER):
            ck = 2.0 ** -(itn + 1)
            P.op("dve", lambda e: e.tensor_scalar(out=cmp2, in0=affE2, scalar1=midc, scalar2=None, op0=ALU.is_ge),
                 reads=["affE2", "midc"], writes=["cmp2"])
            P.op("dve", lambda e: e.reduce_sum(out=cntc, in_=cmp2, axis=AX.X), reads=["cmp2"], writes=["cntc"])
            P.op("dve", lambda e: e.tensor_scalar(out=gec, in0=cntc, scalar1=1023.5, scalar2=-0.5, op0=ALU.is_ge, op1=ALU.add),
                 reads=["cntc"], writes=["gec"])
            P.op("dve", (lambda e, ck=ck: e.scalar_tensor_tensor(out=midc, in0=gec, scalar=ck, in1=midc, op0=ALU.mult, op1=ALU.add)),
                 reads=["gec", "midc"], writes=["midc"])
        P.op("dve", lambda e: e.tensor_scalar(out=loc, in0=midc, scalar1=-(2.0 ** -(NITER + 1)), scalar2=None, op0=ALU.add),
             reads=["midc"], writes=["loc"])
        P.dma(sp, lambda e: e.dma_start(out=thr_d.ap().rearrange("o q -> q o"), in_=loc, allow_slow_non_contiguous=True),
              reads=["loc"], writes=["thr_d"])
        bt = bis
        lo_ = bt[:, 0, 0:16]
        P.dma(sp, lambda e: e.dma_start(out=lo_, in_=thr_d[0:1, :].partition_broadcast(128)), reads=["thr_d"], writes=["lo"])
        thr = lo_
        for t in range(16):
            P.op("dve", (lambda e, t=t: e.tensor_tensor(out=w_tm[:, t, :], in0=aff_tm[:, t, :], in1=thr, op=ALU.is_ge)),
                 reads=[("aff", t), "lo"], writes=[("w", t)])
            P.op("pool", (lambda e, t=t: e.tensor_tensor(out=w_tm[:, t, :], in0=w_tm[:, t, :], in1=aff_tm[:, t, :], op=ALU.mult)),
                 reads=[("aff", t), ("w", t)], writes=[("w", t)])
        P.barrier()
        if STAGE in (2, 3, 4):
            for t in range(16):
                P.dma(sp, (lambda e, t=t: e.dma_start(out=out_d[t * 128:(t + 1) * 128, 0:16], in_=w_tm[:, t, :])),
                      writes=[("out", t)])
                P.dma(sp, (lambda e, t=t: e.dma_start(out=out_d[t * 128:(t + 1) * 128, 16:32], in_=aff_tm[:, t, :])),
                      writes=[("out2", t)])
            P.dma(sp, lambda e: e.dma_start(out=out_d[0:128, 32:64], in_=bis[:, 0, :]), writes=["out3"])
            P.barrier()
            P.emit()
            return nc
        wgv = w_gate.ap().rearrange("e (c p) f -> e p c f", p=128)
        wuv = w_up.ap().rearrange("e (c p) f -> e p c f", p=128)
        step = 0
        for th in range(2):
            P.op("pool", lambda e: e.memset(acc[:, :, :], 0.0), writes=[("acc", i) for i in range(8)])
            for ex in range(NEXP):
                for fb in range(8):
                    bi = step % 2
                    step += 1
                    for (wv, stg_, wbf, nm, ceng) in ((wgv, stgG, wg_bf, "g", "dve"), (wuv, stgU, wu_bf, "u", "pool")):
                        P.dma(sp, (lambda e, wv=wv, stg_=stg_, ex=ex, fb=fb: e.dma_start(
                            out=stg_, in_=wv[ex, :, :, fb * 256:(fb + 1) * 256])), writes=["stg" + nm])
                        P.op(ceng, (lambda e, stg_=stg_, wbf=wbf, bi=bi: e.tensor_copy(out=wbf[:, bi, :, :], in_=stg_)),
                             reads=["stg" + nm], writes=[("w" + nm, bi)])
                    for k2 in range(2):
                        fc = fb * 2 + k2
                        st = stgs[k2]
                        sk = "stg%d" % k2
                        P.dma(sp, (lambda e, st=st, ex=ex, fc=fc: e.dma_start(out=st[:, 0:1024],
                                                                             in_=w_down[ex, fc * 128:(fc + 1) * 128, :])),
                              writes=[sk])
                        P.op("pool" if k2 else "act", (lambda e, st=st, fc=fc, k2=k2: (
                            e.tensor_copy(out=wd_bf[:, fc, :], in_=st[:, 0:1024]) if k2 else
                            e.activation(out=wd_bf[:, fc, :], in_=st[:, 0:1024], func=AF.Copy))),
                            reads=[sk], writes=[("wd", fc)])
                    for k2 in range(2):
                        fc = fb * 2 + k2
                        for nb in range(2):
                            pg_, pu_ = psb[(2 * (k2 * 2 + nb)) % 4], psb[(2 * (k2 * 2 + nb)) % 4 + 1]
                            kg_, ku_ = "ps%d" % ((2 * (k2 * 2 + nb)) % 4), "ps%d" % ((2 * (k2 * 2 + nb)) % 4 + 1)
                            tok0 = th * 1024 + nb * 512
                            for (pp, kk, wbf, nm) in ((pg_, kg_, wg_bf, "g"), (pu_, ku_, wu_bf, "u")):
                                for dc in range(8):
                                    P.op("pe", (lambda e, pp=pp, wbf=wbf, bi=bi, dc=dc, k2=k2, tok0=tok0: e.matmul(
                                        pp[:, :], lhsT=wbf[:, bi, dc, k2 * 128:(k2 + 1) * 128], rhs=h2T[:, dc, tok0:tok0 + 512],
                                        start=(dc == 0), stop=(dc == 7))), reads=[("w" + nm, bi)], writes=[kk])
                            sgt = sgs[nb]
                            sgk = "sg%d" % nb
                            P.op("act", (lambda e, pg_=pg_, sgt=sgt: e.activation(out=sgt, in_=pg_[:, :], func=AF.Silu)),
                                 reads=[kg_], writes=[sgk])
                            P.op("dve", (lambda e, pu_=pu_, sgt=sgt, fc=fc, nb=nb: e.tensor_tensor(
                                out=hidT[:, fc, nb * 512:(nb + 1) * 512], in0=pu_[:, :], in1=sgt, op=ALU.mult)),
                                reads=[ku_, sgk], writes=[("hid", fc)])
                for tl in range(8):
                    for half in range(2):
                        pb = psb[4 + (2 * tl + half) % 3]
                        pk = "ps%d" % (4 + (2 * tl + half) % 3)
                        for fc in range(16):
                            P.op("pe", (lambda e, pb=pb, fc=fc, tl=tl, half=half: e.matmul(
                                pb[:, :], lhsT=hidT[:, fc, tl * 128:(tl + 1) * 128], rhs=wd_bf[:, fc, half * 512:(half + 1) * 512],
                                start=(fc == 0), stop=(fc == 15))), reads=[("hid", fc), ("wd", fc)], writes=[pk])
                        P.op("dve", (lambda e, pb=pb, tl=tl, half=half, th=th, ex=ex: e.scalar_tensor_tensor(
                            out=acc[:, tl, half * 512:(half + 1) * 512], in0=pb[:, :], scalar=w_tm[:, th * 8 + tl, ex:ex + 1],
                            in1=acc[:, tl, half * 512:(half + 1) * 512], op0=ALU.mult, op1=ALU.add)),
                            reads=[pk, ("acc", tl)], writes=[("acc", tl)])
            for tl in range(8):
                t = th * 8 + tl
                xt = xss[t % 2]
                xk = "xs%d" % (t % 2)
                si = 6 + (t % 2)
                ssk = "ss%d" % si
                P.dma(sp, (lambda e, xt=xt, t=t: e.dma_start(out=xt[:, :], in_=x1_d[t * 128:(t + 1) * 128, :])), writes=[xk])
                P.op("dve", (lambda e, tl=tl: e.tensor_tensor(out=acc[:, tl, :], in0=acc[:, tl, :], in1=G2[:, :], op=ALU.mult)),
                     reads=[("acc", tl), "G2"], writes=[("acc", tl)])
                P.op("pool", (lambda e, xt=xt, tl=tl: e.tensor_tensor(out=xt[:, :], in0=xt[:, :], in1=acc[:, tl, :], op=ALU.add)),
                     reads=[xk, ("acc", tl)], writes=[xk])
                P.op("pool", (lambda e, si=si: e.memset(small[:, si:si + 1], 0.0)), writes=[ssk])
                P.op("act", (lambda e, xt=xt, si=si: e.activation(out=junk[:, :], in_=xt[:, :], func=AF.Square,
                                                                  accum_out=small[:, si:si + 1])),
                     reads=[xk, ssk], writes=["junk", ssk])
                P.op("act", (lambda e, si=si: e.activation(out=small[:, si:si + 1], in_=small[:, si:si + 1], func=AF.Sqrt,
                                                           scale=1.0 / D, bias=EPS)), reads=[ssk], writes=[ssk])
                P.op("dve", (lambda e, si=si: e.reciprocal(out=small[:, si:si + 1], in_=small[:, si:si + 1])),
                     reads=[ssk], writes=[ssk])
                P.op("dve", (lambda e, xt=xt, si=si: e.scalar_tensor_tensor(out=xt[:, :], in0=xt[:, :], scalar=small[:, si:si + 1],
                                                                            in1=GF[:, :], op0=ALU.mult, op1=ALU.mult)),
                     reads=[xk, ssk, "GF"], writes=[xk])
                P.dma(sp, (lambda e, xt=xt, t=t: e.dma_start(out=out_d[t * 128:(t + 1) * 128, :], in_=xt[:, :])),
                      reads=[xk], writes=[("out", t)])
    P.barrier()
    P.emit()
    return nc


def _bias_tables(rpb, j):
    row0 = 32 * j
    p = np.arange(128)
    a, kc = p // 64, p % 64
    qq = np.arange(128)
    b_, qc = qq // 64, qq % 64
    c0 = np.clip(qc - 8, 0, 48)
    colv = (kc[:, None] >= c0[None, :]) & (kc[:, None] < c0[None, :] + 16)
    dcol = np.clip(kc[:, None] - qc[None, :] + 15, 0, 30)
    tab = np.full((16, 128, 8, 6, 128), NEG, np.float32)
    for g in range(16):
        lo = min(max(2 * g - 2, 0), 28)
        r = row0 + 2 * g + b_
        r0 = np.clip(r - 4, 0, 120)
        for jt in range(6):
            gkr = row0 + lo + 2 * jt + a - 4
            rowv = (gkr[:, None] >= 0) & (gkr[:, None] < 128) & (gkr[:, None] >= r0[None, :]) & (gkr[:, None] < r0[None, :] + 8)
            valid = rowv & colv
            drow = np.clip(gkr[:, None] - r[None, :] + 7, 0, 14)
            vals = rpb[:, drow, dcol]
            tab[g, :, :, jt, :] = np.where(valid[:, None, :], np.transpose(vals, (1, 0, 2)), np.float32(NEG))
    return tab.reshape(16, 128, 8 * 6 * 128)


_NC_CACHE = {}


def kernel(x, c, ctx, c_ctx, w_mod, b_mod, norm_mix_g, w_in, conv_w, conv_b, conv_ln_g, conv_ln_b,
           rpb, w_out, norm_ffn_g, w_router, w_gate, w_up, w_down, norm_final_g):
    f = lambda a: np.ascontiguousarray(np.asarray(a, dtype=np.float32))
    x, c, ctx, c_ctx = f(x), f(c), f(ctx), f(c_ctx)
    if "nc" not in _NC_CACHE:
        _NC_CACHE["nc"] = build_nc()
    nc = _NC_CACHE["nc"]
    ident = np.eye(128, dtype=np.float32)
    wg_full, wu_full, wd_full = f(w_gate[0][:NEXP]), f(w_up[0][:NEXP]), f(w_down[0][:NEXP])
    in_maps = []
    rpb0 = f(rpb[0])
    tabs = [_bias_tables(rpb0, jj) for jj in range(4)]
    base_tab = np.ascontiguousarray(tabs[1][[0, 1, 14, 15, 2]])
    patch_tab = np.ascontiguousarray(np.stack([tabs[0][0], tabs[0][1], tabs[3][14], tabs[3][15]]))
    for i in range(NCORE):
        b, j = i // 4, i % 4
        order = [jj for jj in range(4) if jj != j] + [j]
        xh = np.zeros((NCH, HALO, D), np.float32)
        tokmask = np.zeros((NCH, 1, HALO), np.float32)
        bsel = np.zeros((128, NCH, 2), np.float32)
        for c4, jj in enumerate(order):
            start = 2048 * jj - 256
            lo, hi = max(start, 0), min(start + HALO, T)
            xh[c4, lo - start:hi - start] = x[b, lo:hi]
            tokmask[c4, 0, lo - start:hi - start] = 1.0
            bsel[:, c4, 0] = 1.0 if jj == 0 else 0.0
            bsel[:, c4, 1] = 1.0 if jj == 3 else 0.0
        in_maps.append({
            "xh": xh, "tokmask": tokmask, "cvec": np.stack([c[b], c_ctx]), "ctx": f(ctx[b]),
            "w_mod": f(w_mod[0]), "b_mod": f(b_mod[0])[None, :], "norm_mix_g": f(norm_mix_g[0])[None, :],
            "norm_ffn_g": f(norm_ffn_g[0])[None, :], "norm_final_g": f(norm_final_g)[None, :],
            "w_in": f(w_in[0]), "conv_w": f(conv_w[0]), "conv_b": f(conv_b[0])[None, :],
            "conv_ln_g": f(conv_ln_g[0])[None, :], "conv_ln_b": f(conv_ln_b[0])[None, :],
            "bias_tab": base_tab, "bias_patch": patch_tab, "bsel": bsel, "w_out": f(w_out[0]), "w_router": f(w_router[0]),
            "w_gate": wg_full, "w_up": wu_full, "w_down": wd_full,
            "ident": ident,
        })
    if STAGE == 4:
        tiny = {"w_mod": [128, 8], "w_in": [128, 8], "bias_tab": [16, 1, 8], "w_gate": [1, 128, 8],
                "w_up": [1, 128, 8], "w_down": [1, 128, 8]}
        for m in in_maps:
            for k, shp in tiny.items():
                m[k] = np.zeros(shp, np.float32)
    if _NC_CACHE.get("prep_only"):
        return in_maps
    res = run_bass_kernel_spmd(nc, in_maps, core_ids=list(range(NCORE)))
    out = np.concatenate([res.results[i]["out"] for i in range(NCORE)], axis=0)
    return out.reshape(2, T, D).astype(np.float32)
```

```python
import numpy as np
import concourse.bass as bass
import concourse.mybir as mybir
from concourse.bass_utils import run_bass_kernel_spmd

F32 = mybir.dt.float32
BF16 = mybir.dt.bfloat16
I32 = mybir.dt.int32
ALU = mybir.AluOpType
AF = mybir.ActivationFunctionType
AX = mybir.AxisListType

D = 1024
T = 8192
NCORE = 8
TOK = 2048
HALO = 2560
NEG = -30000.0
EPS = 1e-6
STAGE = 99
SAME_ENGINE_SYNC = True
NDMA = 24
NEXP = 16
NCH = 4
CUT = 0
SUB = 0
NITER = 26
USE_RELAY = False


class Prog:
    def __init__(self, nc):
        self.nc = nc
        self.q = {k: [] for k in ("pe", "act", "dve", "pool", "sp")}
        self.esem = {k: nc.alloc_semaphore("es_" + k) for k in ("pe", "act", "dve", "pool")}
        self.cnt = {k: 0 for k in self.esem}
        self.dsem = [nc.alloc_semaphore("ds%d" % i) for i in range(NDMA)]
        self.dcnt = [0] * NDMA
        self.rr = 0
        self.ccsem = nc.alloc_semaphore("ccsem")
        self.cccnt = 0
        self.lastw = {}
        self.readers = {}
        self.known = {k: {} for k in self.q}

    def _deps(self, reads, writes):
        deps = []
        for k in reads:
            if k in self.lastw:
                deps.append(self.lastw[k] + ("raw",))
        for k in writes:
            if k in self.lastw:
                deps.append(self.lastw[k] + ("waw",))
            deps.extend(t + ("war",) for t in self.readers.get(k, ()))
        return deps

    def _filter(self, eng, deps):
        out = []
        kn = self.known[eng]
        best = {}
        for (sem, val, src, kind) in deps:
            if src == eng and (USE_RELAY or eng == "pe" or not SAME_ENGINE_SYNC or (kind != "raw" and eng != "pool")):
                continue
            key = id(sem)
            if kn.get(key, 0) >= val:
                continue
            if key not in best or best[key][1] < val:
                best[key] = (sem, val)
        for key, (sem, val) in best.items():
            kn[key] = val
            out.append((sem, val))
        return out

    def _update(self, reads, writes, tok):
        for k in writes:
            self.lastw[k] = tok
            self.readers[k] = []
        for k in reads:
            self.readers.setdefault(k, []).append(tok)

    def _relay(self, eng, deps):
        if eng == "pe" or not SAME_ENGINE_SYNC or not USE_RELAY:
            return deps
        out = []
        need = 0
        for d in deps:
            (sem, val, src, kind) = d
            if src == eng and kind == "raw":
                if self.cnt[eng] - val >= 16:
                    continue
                need = max(need, val)
            else:
                out.append(d)
        if need:
            r = "act" if eng == "pool" else "pool"
            dummy = self.dummy
            if r == "pool":
                fn = lambda e: e.memset(dummy[0:1, 0:1], 0.0)
            else:
                fn = lambda e: e.activation(out=dummy[0:1, 1:2], in_=dummy[0:1, 1:2], func=AF.Copy)
            self.cnt[r] += 1
            tok = (self.esem[r], self.cnt[r], r)
            self.q[r].append((fn, self._filter(r, [(self.esem[eng], need, eng, "x")]), (self.esem[r], 1)))
            out.append(tok + ("raw",))
        return out

    def op(self, eng, fn, reads=(), writes=()):
        deps = self._relay(eng, self._deps(reads, writes))
        self.cnt[eng] += 1
        tok = (self.esem[eng], self.cnt[eng], eng)
        self.q[eng].append((fn, self._filter(eng, deps), (self.esem[eng], 1)))
        self._update(reads, writes, tok)
        return tok

    def dma(self, queue, fn, reads=(), writes=()):
        deps = self._deps(reads, writes)
        k = self.rr
        self.rr = (k + 1) % NDMA
        sem = self.dsem[k]
        if self.dcnt[k] > 0:
            deps.append((sem, self.dcnt[k], "dma", "raw"))
        self.dcnt[k] += 16
        tok = (sem, self.dcnt[k], "dma")
        self.q[queue].append((fn, self._filter(queue, deps), (sem, 16)))
        self._update(reads, writes, tok)
        return tok

    def cc(self, fn, reads=(), writes=()):
        deps = self._deps(reads, writes)
        self.cccnt += 1
        tok = (self.ccsem, self.cccnt, "cc")
        self.q["pool"].append((fn, self._filter("pool", deps), (self.ccsem, 1)))
        self._update(reads, writes, tok)
        return tok

    def snapshot(self):
        import copy
        return dict(ql={k: len(v) for k, v in self.q.items()}, cnt=dict(self.cnt), dcnt=list(self.dcnt), rr=self.rr,
                    cccnt=self.cccnt, lastw=dict(self.lastw), readers={k: list(v) for k, v in self.readers.items()},
                    known={k: dict(v) for k, v in self.known.items()})

    def restore(self, sn):
        for k in self.q:
            del self.q[k][sn["ql"][k]:]
        self.cnt = dict(sn["cnt"]); self.dcnt = list(sn["dcnt"]); self.rr = sn["rr"]; self.cccnt = sn["cccnt"]
        self.lastw = dict(sn["lastw"]); self.readers = {k: list(v) for k, v in sn["readers"].items()}
        self.known = {k: dict(v) for k, v in sn["known"].items()}

    def barrier(self):
        toks = [(self.esem[k], self.cnt[k], k) for k in self.esem if self.cnt[k] > 0]
        toks += [(self.dsem[i], self.dcnt[i], "dma") for i in range(NDMA) if self.dcnt[i] > 0]
        if self.cccnt:
            toks.append((self.ccsem, self.cccnt, "cc"))
        for eng in self.q:
            deps = [t for t in toks if t[2] != eng or eng != "pe"]
            w = []
            kn = self.known[eng]
            for (sem, val, src) in deps:
                if kn.get(id(sem), 0) >= val:
                    continue
                kn[id(sem)] = val
                w.append((sem, val))
            if w:
                self.q[eng].append((None, w, None))
        self.lastw = {}
        self.readers = {}

    def emit(self):
        nc = self.nc
        q = self.q

        def replay(e, name):
            for (fn, waits, inc) in q[name]:
                for (sem, val) in waits:
                    e.wait_ge(sem, val)
                if fn is not None:
                    ins = fn(e)
                    ins.then_inc(inc[0], inc[1])

        with nc.Block() as block:
            @block.tensor
            def _(e):
                replay(e, "pe")

            @block.scalar
            def _(e):
                replay(e, "act")

            @block.vector
            def _(e):
                replay(e, "dve")

            @block.gpsimd
            def _(e):
                replay(e, "pool")

            @block.sync
            def _(e):
                replay(e, "sp")


def build_nc():
    nc = bass.Bass("TRN2", target_bir_lowering=False)
    P = Prog(nc)
    P.dummy = nc.alloc_sbuf_tensor("s_dummy", [1, 8], F32)

    TINY = {"w_mod": [128, 8], "w_in": [128, 8], "bias_tab": [16, 1, 8], "w_gate": [1, 128, 8],
            "w_up": [1, 128, 8], "w_down": [1, 128, 8]}

    def din(name, shape, dt=F32):
        if STAGE == 4 and name in TINY:
            shape = TINY[name]
        return nc.dram_tensor(name, list(shape), dt, kind="ExternalInput")

    xh = din("xh", [NCH, HALO, D])
    tokmask = din("tokmask", [NCH, 1, HALO])
    cvec = din("cvec", [2, D])
    ctx_d = din("ctx", [256, D])
    w_mod = din("w_mod", [D, 6 * D])
    b_mod = din("b_mod", [1, 6 * D])
    g_mix = din("norm_mix_g", [1, D])
    g_ffn = din("norm_ffn_g", [1, D])
    g_fin = din("norm_final_g", [1, D])
    w_in = din("w_in", [D, 2560])
    conv_w = din("conv_w", [31, 512])
    conv_b = din("conv_b", [1, 512])
    cln_g = din("conv_ln_g", [1, 512])
    cln_b = din("conv_ln_b", [1, 512])
    bias_tab = din("bias_tab", [5, 128, 8 * 6 * 128])
    bias_patch = din("bias_patch", [4, 128, 8 * 6 * 128])
    bsel_d = din("bsel", [128, NCH, 2])
    w_out = din("w_out", [D, D])
    w_router = din("w_router", [D, 16])
    w_gate = din("w_gate", [NEXP, D, 2048])
    w_up = din("w_up", [NEXP, D, 2048])
    w_down = din("w_down", [NEXP, 2048, D])
    ident_d = din("ident", [128, 128])
    out_d = nc.dram_tensor("out", [TOK, D], F32, kind="ExternalOutput")

    mod_d = nc.dram_tensor("mod_d", [2, 6 * D], F32)
    x1_d = nc.dram_tensor("x1_d", [TOK, D], F32)
    thr_d = nc.dram_tensor("thr_d", [1, 16], F32)
    aff_loc = nc.dram_tensor("aff_loc", [NCH, 16, TOK], F32)

    def sb(name, shape, dt):
        return nc.alloc_sbuf_tensor("s_" + name, list(shape), dt)

    def ps(name, shape, dt=F32):
        return nc.alloc_psum_tensor("p_" + name, list(shape), dt)

    ident = sb("ident", [128, 128], F32)
    ident_bf = sb("ident_bf", [128, 128], BF16)
    ones_bf = sb("ones_bf", [128, 128], BF16)
    maskb = sb("maskb", [128, 32], F32)
    cT = sb("cT", [128, 8, 2], F32)
    ones2 = sb("ones2", [1, 2], F32)
    G1 = sb("G1", [128, D], F32)
    A2 = sb("A2", [128, D], F32)
    B2 = sb("B2", [128, D], F32)
    G2 = G1
    GF = A2
    chv = sb("chv", [128, 8, 6], F32)
    A1T = sb("A1T", [128, 8], F32)
    cA1T = sb("cA1T", [128, 8], F32)
    cvp = sb("cvp", [128, 4, 3], F32)
    convwT = sb("convwT", [128, 4, 31], F32)
    small = sb("small", [128, 64], F32)
    aff_tm = sb("aff_tm", [128, 16, 16], F32)
    w_tm = sb("w_tm", [128, 16, 16], F32)
    wr = sb("wr", [128, 8, 16], F32)
    bis = sb("bis", [128, 8, 32], F32)
    cnt_bf = sb("cnt_bf", [128, 32], BF16)
    bcol = sb("bcol", [32, 4], F32)
    bselt = sb("bselt", [128, NCH, 4], F32)

    arenaA = sb("arenaA", [128, 40960], BF16)
    arenaB = sb("arenaB", [128, 41280], BF16)
    stg0 = sb("stg0", [128, 1536], F32)
    stg1 = sb("stg1", [128, 1536], F32)
    xs0 = sb("xs0", [128, D], F32)
    xs1 = sb("xs1", [128, D], F32)
    junk = sb("junk", [128, D], BF16)

    psb = [ps("psb%d" % i, [128, 512], F32) for i in range(7)]
    pstr = ps("pstr", [128, 1024], BF16)

    arenaA_f = arenaA[:, :].bitcast(F32)
    arenaB_f = arenaB[:, :].bitcast(F32)
    hT = arenaA[:, 0:20480].rearrange("p (c t) -> p c t", c=8)
    w_bf = arenaA[:, 20480:40960].rearrange("p (c t) -> p c t", c=8)
    o = 0
    vT = arenaB[:, o:o + 4 * 2080].rearrange("p (c t) -> p c t", c=4); o += 4 * 2080
    qT = arenaB[:, o:o + 4 * 2048].rearrange("p (c t) -> p c t", c=4); o += 4 * 2048
    kT = arenaB[:, o:o + 4 * 2560].rearrange("p (c t) -> p c t", c=4); o += 4 * 2560
    v_tm = arenaB[:, o:o + 20 * 520].rearrange("p (t h e) -> p t h e", t=20, h=8); o += 20 * 520
    kcT = arenaB[:, o:o + 4 * 256].rearrange("p (c t) -> p c t", c=4); o += 4 * 256
    vc_tm = arenaB[:, o:o + 2 * 520].rearrange("p (t h e) -> p t h e", t=2, h=8); o += 2 * 520
    hcT = arenaB[:, o:o + 8 * 256].rearrange("p (c t) -> p c t", c=8); o += 8 * 256
    assert o <= 41280, o

    sp = "sp"

    P.dma(sp, lambda e: e.dma_start(out=ident[:, :], in_=ident_d[:, :]), writes=["ident"])
    P.op("dve", lambda e: e.memset(ones2[:, :], 1.0), writes=["ones2"])
    P.dma(sp, lambda e: e.dma_start(out=bselt[:, :, 0:2], in_=bsel_d[:, :, :]), writes=["bsel"])
    P.op("dve", lambda e: e.tensor_scalar(out=bselt[:, :, 2:4], in0=bselt[:, :, 0:2], scalar1=-1.0, scalar2=1.0,
                                          op0=ALU.mult, op1=ALU.add), reads=["bsel"], writes=["bsel2"])
    P.op("dve", lambda e: e.memset(P.dummy[:, :], 0.0), writes=["dummy"])
    for r in range(2):
        P.dma(sp, (lambda e, r=r: e.dma_start(out=cT[:, :, r:r + 1], in_=cvec[r:r + 1, :].rearrange("r (c p) -> p c r", p=128),
                                              allow_slow_non_contiguous=True)), writes=["cT"])
    P.op("dve", lambda e: e.tensor_copy(out=ident_bf[:, :], in_=ident[:, :]), reads=["ident"], writes=["ident_bf"])
    P.op("dve", lambda e: e.memset(ones_bf[:, :], 1.0), writes=["ones_bf"])
    P.op("act", lambda e: e.activation(out=cT[:, :, :], in_=cT[:, :, :], func=AF.Silu), reads=["cT"], writes=["cT"])

    snap0 = P.snapshot()
    stgs = [stg0, stg1]
    mrow = stg0[0:2, 1024:1536]
    bmc = stg1[0:1, 1024:1536]
    wm_v = w_mod.ap().rearrange("(c p) n -> p c n", p=128)
    ld = 0
    for n in range(12):
        P.dma(sp, (lambda e, n=n: e.dma_start(out=bmc, in_=b_mod[0:1, n * 512:(n + 1) * 512])),
              writes=["bmc"])
        for q4 in range(4):
            st = stgs[ld % 2]
            key = "stg%d" % (ld % 2)
            ld += 1
            stv = st[:, 0:1024].rearrange("p (c n) -> p c n", c=2)
            P.dma(sp, (lambda e, stv=stv, n=n, q4=q4: e.dma_start(
                out=stv, in_=wm_v[:, q4 * 2:q4 * 2 + 2, n * 512:(n + 1) * 512])), writes=[key])
            for c2 in range(2):
                c = q4 * 2 + c2
                P.op("pe", (lambda e, stv=stv, c2=c2, c=c: e.matmul(
                    psb[0][0:2, :], lhsT=cT[:, c, :], rhs=stv[:, c2, :], start=(c == 0), stop=False)),
                    reads=[key, "cT"], writes=["ps0"])
        P.op("pe", (lambda e, n=n: e.matmul(psb[0][0:2, :], lhsT=ones2[:, :], rhs=bmc, start=False, stop=True)),
             reads=["bmc", "ones2"], writes=["ps0"])
        P.op("dve", (lambda e, n=n: e.tensor_copy(out=mrow, in_=psb[0][0:2, :])),
             reads=["ps0"], writes=["mrow"])
        P.dma(sp, (lambda e, n=n: e.dma_start(out=mod_d[:, n * 512:(n + 1) * 512], in_=mrow)),
              reads=["mrow"], writes=["mod_d"])

    def bc(dst, src_ap, key):
        P.dma(sp, lambda e: e.dma_start(out=dst[:, :], in_=src_ap.partition_broadcast(128)),
              reads=["mod_d"], writes=[key])

    bc(G1, mod_d[0:1, 2 * D:3 * D], "G1")
    bc(B2, mod_d[0:1, 3 * D:4 * D], "B2")
    bc(A2, mod_d[0:1, 4 * D:5 * D], "A2")
    bc(xs0, g_ffn[0:1, :], "xs0")
    P.op("dve", lambda e: e.scalar_tensor_tensor(out=A2[:, :], in0=A2[:, :], scalar=1.0, in1=xs0[:, :],
                                                 op0=ALU.add, op1=ALU.mult), reads=["A2", "xs0"], writes=["A2"])

    def chload(idx, src_ap):
        P.dma(sp, lambda e: e.dma_start(out=chv[:, :, idx:idx + 1], in_=src_ap.rearrange("r (c p) -> p c r", p=128),
                                        allow_slow_non_contiguous=True), reads=["mod_d"], writes=["chv%d" % idx])

    chload(0, mod_d[0:1, 0:D])
    chload(1, mod_d[0:1, D:2 * D])
    chload(2, mod_d[1:2, 0:D])
    chload(3, mod_d[1:2, D:2 * D])
    chload(4, g_mix[0:1, :])
    P.op("dve", lambda e: e.scalar_tensor_tensor(out=A1T[:, :], in0=chv[:, :, 1], scalar=1.0, in1=chv[:, :, 4],
                                                 op0=ALU.add, op1=ALU.mult), reads=["chv1", "chv4"], writes=["A1T"])
    P.op("dve", lambda e: e.scalar_tensor_tensor(out=cA1T[:, :], in0=chv[:, :, 3], scalar=1.0, in1=chv[:, :, 4],
                                                 op0=ALU.add, op1=ALU.mult), reads=["chv3", "chv4"], writes=["cA1T"])
    for i, src in enumerate((conv_b, cln_g, cln_b)):
        P.dma(sp, (lambda e, i=i, src=src: e.dma_start(out=cvp[:, :, i:i + 1],
                                                       in_=src.ap().rearrange("r (c p) -> p c r", p=128),
                                                       allow_slow_non_contiguous=True)), writes=["cvp%d" % i])
    for cc in range(4):
        P.dma(sp, (lambda e, cc=cc: e.dma_start(out=convwT[:, cc, :], in_=conv_w[:, cc * 128:(cc + 1) * 128].rearrange("j p -> p j"),
                                                allow_slow_non_contiguous=True)), writes=["convwT"])

    for ch in range(NCH):
        P.dma(sp, (lambda e, ch=ch: e.dma_start(out=maskb[:, 0:16], in_=tokmask[ch, 0:1, 240:256].partition_broadcast(128))), writes=["maskb"])
        P.dma(sp, (lambda e, ch=ch: e.dma_start(out=maskb[:, 16:32], in_=tokmask[ch, 0:1, 2304:2320].partition_broadcast(128))), writes=["maskb"])
        snapA = P.snapshot()
        xss = [xs0, xs1]

        def norm_tile(src_ap, xi, dstT, t0, AT, BT_idx, tag):
            xt = xss[xi]
            xk = "xs%d" % xi
            P.dma(sp, lambda e: e.dma_start(out=xt[:, :], in_=src_ap), writes=[xk])
            P.op("pool", lambda e: e.memset(small[:, xi:xi + 1], 0.0), writes=["ss%d" % xi])
            P.op("act", lambda e: e.activation(out=junk[:, :], in_=xt[:, :], func=AF.Square, accum_out=small[:, xi:xi + 1]),
                 reads=[xk, "ss%d" % xi], writes=["junk", "ss%d" % xi])
            P.op("act", lambda e: e.activation(out=small[:, xi:xi + 1], in_=small[:, xi:xi + 1], func=AF.Sqrt,
                                               scale=1.0 / D, bias=EPS), reads=["ss%d" % xi], writes=["ss%d" % xi])
            P.op("dve", lambda e: e.reciprocal(out=small[:, xi:xi + 1], in_=small[:, xi:xi + 1]),
                 reads=["ss%d" % xi], writes=["ss%d" % xi])
            P.op("dve", lambda e: e.tensor_scalar(out=xt[:, :], in0=xt[:, :], scalar1=small[:, xi:xi + 1], scalar2=None,
                                                  op0=ALU.mult), reads=[xk, "ss%d" % xi], writes=[xk])
            for half in range(2):
                pb = psb[1 + half]
                pk = "ps%d" % (1 + half)
                for c4 in range(4):
                    dc = half * 4 + c4
                    P.op("pe", (lambda e, pb=pb, c4=c4, dc=dc: e.transpose(out=pb[:, c4 * 128:(c4 + 1) * 128],
                                                                           in_=xt[:, dc * 128:(dc + 1) * 128],
                                                                           identity=ident[:, :])),
                         reads=[xk, "ident"], writes=[pk])
                for c4 in range(4):
                    dc = half * 4 + c4
                    P.op("act", (lambda e, pb=pb, c4=c4, dc=dc: e.activation(
                        out=dstT[:, dc, t0:t0 + 128], in_=pb[:, c4 * 128:(c4 + 1) * 128], func=AF.Identity,
                        scale=AT[:, dc:dc + 1], bias=chv[:, dc, BT_idx:BT_idx + 1])),
                        reads=[pk, "A1T", "cA1T", "chv0", "chv2"], writes=[(tag, t0 // 128)])

        for tt in range(20):
            norm_tile(xh[ch, tt * 128:(tt + 1) * 128, :], tt % 2, hT, tt * 128, A1T, 0, "hT")
        for tt in range(2):
            norm_tile(ctx_d[tt * 128:(tt + 1) * 128, :], tt % 2, hcT, tt * 128, cA1T, 2, "hcT")

        for dc in range(8):
            for hf in range(2):
                st = stgs[hf]
                key = "stg%d" % hf
                P.dma(sp, (lambda e, st=st, dc=dc, hf=hf: e.dma_start(out=st[:, 0:1280],
                                                                     in_=w_in[dc * 128:(dc + 1) * 128, hf * 1280:(hf + 1) * 1280])),
                      writes=[key])
                P.op("pool" if hf else "dve", (lambda e, st=st, dc=dc, hf=hf: e.tensor_copy(
                    out=w_bf[:, dc, hf * 1280:(hf + 1) * 1280], in_=st[:, 0:1280])), reads=[key], writes=[("w_bf", dc, hf)])
        wkeys = [("w_bf", dc, hf) for dc in range(8) for hf in range(2)]
        hkeys = lambda t0, n: [("hT", k) for k in range(t0 // 128, (t0 + n + 127) // 128)]

        def mm_chan(pb, pk, col0, t0, n, src=hT, skeys=None):
            for dc in range(8):
                P.op("pe", (lambda e, dc=dc: e.matmul(pb[:, 0:n], lhsT=w_bf[:, dc, col0:col0 + 128],
                                                       rhs=src[:, dc, t0:t0 + n], start=(dc == 0), stop=(dc == 7))),
                     reads=wkeys + (skeys if skeys is not None else hkeys(t0, n)), writes=[pk])

        ublocks = [(240, 512), (752, 512), (1264, 512), (1776, 512), (2288, 32)]
        it = 0
        for (t0, n) in ublocks:
            for cc in range(4):
                pa, pg = psb[3 + 2 * (it % 2)], psb[4 + 2 * (it % 2)]
                ka, kg = "ps%d" % (3 + 2 * (it % 2)), "ps%d" % (4 + 2 * (it % 2))
                st = stgs[it % 2]
                sk = "stg%d" % (it % 2)
                it += 1
                mm_chan(pa, ka, cc * 128, t0, n)
                mm_chan(pg, kg, 512 + cc * 128, t0, n)
                P.op("act", (lambda e, pg=pg, st=st, n=n: e.activation(out=st[:, 0:n], in_=pg[:, 0:n], func=AF.Sigmoid)),
                     reads=[kg], writes=[sk])
                P.op("dve", (lambda e, pa=pa, st=st, n=n, t0=t0, cc=cc: e.tensor_tensor(
                    out=vT[:, cc, t0 - 240:t0 - 240 + n], in0=pa[:, 0:n], in1=st[:, 0:n], op=ALU.mult)),
                    reads=[ka, sk], writes=[("vT", cc)])
        for cc in range(4):
            P.op("dve", (lambda e, cc=cc: e.tensor_tensor(out=vT[:, cc, 0:16], in0=vT[:, cc, 0:16], in1=maskb[:, 0:16], op=ALU.mult)),
                 reads=[("vT", cc), "maskb"], writes=[("vT", cc)])
            P.op("dve", (lambda e, cc=cc: e.tensor_tensor(out=vT[:, cc, 2064:2080], in0=vT[:, cc, 2064:2080], in1=maskb[:, 16:32],
                                                          op=ALU.mult)), reads=[("vT", cc), "maskb"], writes=[("vT", cc)])
        for tb in range(4):
            for cc in range(4):
                pb, pk = psb[3 + (it % 4)], "ps%d" % (3 + (it % 4))
                it += 1
                mm_chan(pb, pk, 1024 + cc * 128, 256 + tb * 512, 512)
                P.op("act", (lambda e, pb=pb, cc=cc, tb=tb: e.activation(out=qT[:, cc, tb * 512:(tb + 1) * 512], in_=pb[:, :],
                                                                         func=AF.Copy, scale=0.125)),
                     reads=[pk], writes=[("qT", cc)])
        for tb in range(5):
            for cc in range(4):
                pb, pk = psb[3 + (it % 4)], "ps%d" % (3 + (it % 4))
                it += 1
                mm_chan(pb, pk, 1536 + cc * 128, tb * 512, 512)
                P.op("dve", (lambda e, pb=pb, cc=cc, tb=tb: e.tensor_copy(out=kT[:, cc, tb * 512:(tb + 1) * 512], in_=pb[:, :])),
                     reads=[pk], writes=[("kT", cc)])
        for cc in range(4):
            pb, pk = psb[3 + (it % 4)], "ps%d" % (3 + (it % 4))
            it += 1
            mm_chan(pb, pk, 1536 + cc * 128, 0, 256, src=hcT, skeys=[("hcT", 0), ("hcT", 1)])
            P.op("dve", (lambda e, pb=pb, cc=cc: e.tensor_copy(out=kcT[:, cc, :], in_=pb[:, 0:256])),
                 reads=[pk], writes=[("kcT", cc)])
        P.op("pool", lambda e: e.memset(v_tm[:, :, :, 64:65], 1.0), writes=["v_ones"])
        P.op("pool", lambda e: e.memset(vc_tm[:, :, :, 64:65], 1.0), writes=["vc_ones"])

        def mm_v(pb, pk, src, t0, skeys):
            for dc in range(8):
                P.op("pe", (lambda e, dc=dc: e.matmul(pb[:, :], lhsT=src[:, dc, t0:t0 + 128], rhs=w_bf[:, dc, 2048:2560],
                                                       start=(dc == 0), stop=(dc == 7))),
                     reads=wkeys + skeys, writes=[pk])

        for tt in range(20):
            pb, pk = psb[3 + (it % 4)], "ps%d" % (3 + (it % 4))
            it += 1
            mm_v(pb, pk, hT, tt * 128, [("hT", tt)])
            P.op("act" if tt % 2 else "dve", (lambda e, pb=pb, tt=tt: (e.activation(
                out=v_tm[:, tt, :, 0:64], in_=pb[:, :].rearrange("p (h e) -> p h e", h=8), func=AF.Copy)
                if tt % 2 else e.tensor_copy(out=v_tm[:, tt, :, 0:64], in_=pb[:, :].rearrange("p (h e) -> p h e", h=8)))),
                reads=[pk], writes=[("v_tm", tt)])
        for tt in range(2):
            pb, pk = psb[3 + (it % 4)], "ps%d" % (3 + (it % 4))
            it += 1
            mm_v(pb, pk, hcT, tt * 128, [("hcT", tt)])
            P.op("dve", (lambda e, pb=pb, tt=tt: e.tensor_copy(out=vc_tm[:, tt, :, 0:64],
                                                               in_=pb[:, :].rearrange("p (h e) -> p h e", h=8))),
                 reads=[pk], writes=[("vc_tm", tt)])

        P.barrier()

        diag = arenaA[:, 0:124 * 128].rearrange("p (j m) -> p j m", m=128)
        yc = arenaA[:, 15872:15872 + 2048].rearrange("p (c t) -> p c t", c=4)
        ysq = arenaA[:, 17920:17920 + 2048].rearrange("p (c t) -> p c t", c=4)
        yT = arenaA[:, 24576:40960].rearrange("p (c t) -> p c t", c=8)
        for j in range(31):
            for cc in range(4):
                P.op("dve" if (j + cc) % 2 else "pool", (lambda e, j=j, cc=cc: e.tensor_scalar(
                    out=diag[:, j * 4 + cc, :], in0=ident[:, :], scalar1=convwT[:, cc, j:j + 1], scalar2=None, op0=ALU.mult)),
                    reads=["ident", "convwT"], writes=[("diag", j, cc)])
        it = 0
        for tb in range(4):
            for cc in range(4):
                pb, pk = psb[it % 4], "ps%d" % (it % 4)
                it += 1
                for j in range(31):
                    P.op("pe", (lambda e, pb=pb, j=j, cc=cc, tb=tb: e.matmul(
                        pb[:, :], lhsT=diag[:, j * 4 + cc, :], rhs=vT[:, cc, tb * 512 + j + 1:tb * 512 + j + 513],
                        start=(j == 0), stop=(j == 30))), reads=[("diag", j, cc), ("vT", cc)], writes=[pk])
                P.op("act", (lambda e, pb=pb, cc=cc, tb=tb: e.activation(out=yc[:, cc, :], in_=pb[:, :],
                                                                         func=AF.Identity, bias=cvp[:, cc, 0:1])),
                     reads=[pk, "cvp0"], writes=[("yc", cc)])
                P.op("act", (lambda e, pb=pb, cc=cc, tb=tb: e.activation(out=ysq[:, cc, :], in_=pb[:, :],
                                                                         func=AF.Square, bias=cvp[:, cc, 0:1])),
                     reads=[pk, "cvp0"], writes=[("ysq", cc)])
            for cc in range(4):
                P.op("pe", (lambda e, cc=cc, tb=tb: e.matmul(psb[4][:, :], lhsT=ones_bf[:, :], rhs=yc[:, cc, :],
                                                              start=(cc == 0), stop=(cc == 3))),
                     reads=["ones_bf", ("yc", cc)], writes=["ps4"])
            for cc in range(4):
                P.op("pe", (lambda e, cc=cc, tb=tb: e.matmul(psb[5][:, :], lhsT=ones_bf[:, :], rhs=ysq[:, cc, :],
                                                              start=(cc == 0), stop=(cc == 3))),
                     reads=["ones_bf", ("ysq", cc)], writes=["ps5"])
            mean = stg0[:, 0:512]
            rstd = stg0[:, 512:1024]
            tmp = stg0[:, 1024:1536]
            P.op("dve", lambda e: e.tensor_scalar(out=mean, in0=psb[4][:, :], scalar1=1.0 / 512, scalar2=None, op0=ALU.mult),
                 reads=["ps4"], writes=["mean"])
            P.op("dve", lambda e: e.tensor_tensor(out=tmp, in0=mean, in1=mean, op=ALU.mult), reads=["mean"], writes=["tmp"])
            P.op("dve", lambda e: e.scalar_tensor_tensor(out=rstd, in0=psb[5][:, :], scalar=1.0 / 512, in1=tmp,
                                                         op0=ALU.mult, op1=ALU.subtract), reads=["ps5", "tmp"], writes=["rstd"])
            P.op("act", lambda e: e.activation(out=rstd, in_=rstd, func=AF.Sqrt, bias=EPS), reads=["rstd"], writes=["rstd"])
            P.op("dve", lambda e: e.reciprocal(out=rstd, in_=rstd), reads=["rstd"], writes=["rstd"])
            for cc in range(4):
                t1 = stg1[:, (cc % 2) * 512:(cc % 2) * 512 + 512]
                tk = "t1_%d" % (cc % 2)
                P.op("dve", (lambda e, cc=cc, tb=tb, t1=t1: e.tensor_tensor(out=t1, in0=yc[:, cc, :],
                                                                             in1=mean, op=ALU.subtract)),
                     reads=[("yc", cc), "mean"], writes=[tk])
                P.op("dve", (lambda e, t1=t1: e.tensor_tensor(out=t1, in0=t1, in1=rstd, op=ALU.mult)),
                     reads=[tk, "rstd"], writes=[tk])
                P.op("act", (lambda e, cc=cc, tb=tb, t1=t1: e.activation(out=yT[:, cc, tb * 512:(tb + 1) * 512], in_=t1,
                                                                         func=AF.Silu, scale=cvp[:, cc, 1:2], bias=cvp[:, cc, 2:3])),
                     reads=[tk, "cvp1", "cvp2"], writes=[("yT", cc, tb)])
        P.barrier()

        bstg = arenaA_f[:, 0:6144]
        EBs = [arenaA[:, 12288:18432], arenaA[:, 18432:24576]]
        stg1_bf = stg1[:, :].bitcast(BF16)
        pts = [stg1_bf[:, 0:1024], stg1_bf[:, 1024:2048]]
        yatts = [stg1_bf[:, 2048:2560], stg1_bf[:, 2560:3072]]
        rec = small[:, 8:24]
        ps_s = [psb[0], psb[1], psb[2], psb[3]]
        def emit_S(g, h):
            lo = min(max(2 * g - 2, 0), 28)
            cc, pofs = h // 2, (h % 2) * 64
            sA, sB = (psb[0], psb[1]) if h % 2 == 0 else (psb[2], psb[3])
            kA, kB = ("ps0", "ps1") if h % 2 == 0 else ("ps2", "ps3")
            Q = qT[pofs:pofs + 64, cc, g * 128:(g + 1) * 128]
            for jt in range(6):
                dst = sA[:, jt * 128:(jt + 1) * 128] if jt < 4 else sB[:, (jt - 4) * 128:(jt - 3) * 128]
                t0 = 64 * lo + jt * 128
                P.op("pe", (lambda e, dst=dst, t0=t0, cc=cc, pofs=pofs, Q=Q: e.matmul(
                    dst, lhsT=kT[pofs:pofs + 64, cc, t0:t0 + 128], rhs=Q, start=True, stop=True)),
                    reads=[("kT", cc), ("qT", cc)], writes=[kA if jt < 4 else kB])
            for jc in range(2):
                dst = sB[:, (2 + jc) * 128:(3 + jc) * 128]
                P.op("pe", (lambda e, dst=dst, jc=jc, cc=cc, pofs=pofs, Q=Q: e.matmul(
                    dst, lhsT=kcT[pofs:pofs + 64, cc, jc * 128:(jc + 1) * 128], rhs=Q, start=True, stop=True)),
                    reads=[("kcT", cc), ("qT", cc)], writes=[kB])

        emit_S(0, 0)
        for g in range(16):
            lo = min(max(2 * g - 2, 0), 28)
            EB = EBs[g % 2]
            ek = "EB%d" % (g % 2)
            gi = {0: 0, 1: 1, 14: 2, 15: 3}.get(g, 4)
            P.dma(sp, (lambda e, gi=gi: e.dma_start(out=bstg, in_=bias_tab[gi, :, :])), writes=["bstg"])
            if gi < 4:
                si = 0 if gi < 2 else 1
                for hf in range(2):
                    tmpH = arenaB_f[:, 0:3072]
                    P.dma(sp, (lambda e, gi=gi, hf=hf: e.dma_start(out=tmpH, in_=bias_patch[gi, :, hf * 3072:(hf + 1) * 3072])),
                          writes=["tmpH"])
                    P.op("dve", (lambda e, hf=hf, si=si, ch=ch: e.tensor_scalar(
                        out=bstg[:, hf * 3072:(hf + 1) * 3072], in0=bstg[:, hf * 3072:(hf + 1) * 3072],
                        scalar1=bselt[:, ch, 2 + si:3 + si], scalar2=None, op0=ALU.mult)), reads=["bstg", "bsel2"], writes=["bstg"])
                    P.op("dve", (lambda e, hf=hf, si=si, ch=ch: e.scalar_tensor_tensor(
                        out=bstg[:, hf * 3072:(hf + 1) * 3072], in0=tmpH, scalar=bselt[:, ch, si:si + 1],
                        in1=bstg[:, hf * 3072:(hf + 1) * 3072], op0=ALU.mult, op1=ALU.add)),
                        reads=["bstg", "tmpH", "bsel"], writes=["bstg"])
            for q4 in range(4):
                P.op("act", (lambda e, EB=EB, q4=q4: e.activation(out=EB[:, q4 * 1536:(q4 + 1) * 1536],
                                                                  in_=bstg[:, q4 * 1536:(q4 + 1) * 1536], func=AF.Exp)),
                     reads=["bstg"], writes=[(ek, q4)])
            po_t = [psb[4], psb[5]]
            for h in range(8):
                cc, pofs = h // 2, (h % 2) * 64
                sA, sB = (psb[0], psb[1]) if h % 2 == 0 else (psb[2], psb[3])
                kA, kB = ("ps0", "ps1") if h % 2 == 0 else ("ps2", "ps3")
                pt = pts[h % 2]
                ptk = "pt%d" % (h % 2)
                Q = qT[pofs:pofs + 64, cc, g * 128:(g + 1) * 128]
                nxt = (g, h + 1) if h < 7 else ((g + 1, 0) if g < 15 else None)
                if nxt is not None:
                    emit_S(*nxt)
                P.op("act", (lambda e, sA=sA, pt=pt: e.activation(out=pt[:, 0:512], in_=sA[:, :], func=AF.Exp)),
                     reads=[kA], writes=[ptk + "a"])
                P.op("act", (lambda e, sB=sB, pt=pt: e.activation(out=pt[:, 512:1024], in_=sB[:, :], func=AF.Exp)),
                     reads=[kB], writes=[ptk + "b"])
                P.op("dve", (lambda e, pt=pt, EB=EB, h=h: e.tensor_tensor(out=pt[:, 0:512], in0=pt[:, 0:512],
                                                                           in1=EB[:, h * 768:h * 768 + 512], op=ALU.mult)),
                     reads=[ptk + "a", (ek, (h * 768) // 1536), (ek, (h * 768 + 511) // 1536)], writes=[ptk + "a"])
                P.op("dve", (lambda e, pt=pt, EB=EB, h=h: e.tensor_tensor(out=pt[:, 512:768], in0=pt[:, 512:768],
                                                                           in1=EB[:, h * 768 + 512:h * 768 + 768], op=ALU.mult)),
                     reads=[ptk + "b", (ek, (h * 768 + 512) // 1536), (ek, (h * 768 + 767) // 1536)], writes=[ptk + "b"])
                po = po_t[h // 4]
                pok = "ps%d" % (4 + h // 4)
                hh = h % 4
                for jt in range(8):
                    if jt < 6:
                        rhs = v_tm[:, lo // 2 + jt, h, :]
                        rk = ("v_tm", lo // 2 + jt)
                    else:
                        rhs = vc_tm[:, jt - 6, h, :]
                        rk = ("vc_tm", jt - 6)
                    P.op("pe", (lambda e, po=po, hh=hh, jt=jt, pt=pt, rhs=rhs: e.matmul(
                        po[:, hh * 65:(hh + 1) * 65], lhsT=pt[:, jt * 128:(jt + 1) * 128], rhs=rhs,
                        start=(jt == 0), stop=(jt == 7))),
                        reads=[ptk + ("a" if jt < 4 else "b"), rk, "v_ones", "vc_ones"], writes=[pok])
            yatt = yatts[g % 2]
            yk = "yatt%d" % (g % 2)
            for half in range(2):
                po = po_t[half]
                pok = "ps%d" % (4 + half)
                pov = po[:, 0:260].rearrange("p (h e) -> p h e", h=4)
                P.op("dve", (lambda e, pov=pov, half=half: e.reciprocal(out=rec[:, half * 4:half * 4 + 4], in_=pov[:, :, 64])),
                     reads=[pok], writes=["rec%d" % half])
                for hh in range(4):
                    h = half * 4 + hh
                    if hh % 2 == 0:
                        P.op("act", (lambda e, pov=pov, hh=hh, h=h, yatt=yatt: e.activation(
                            out=yatt[:, h * 64:(h + 1) * 64], in_=pov[:, hh, 0:64], func=AF.Copy, scale=rec[:, h:h + 1])),
                            reads=[pok, "rec%d" % half], writes=[(yk, h)])
                    else:
                        P.op("dve", (lambda e, pov=pov, hh=hh, h=h, yatt=yatt: e.tensor_scalar(
                            out=yatt[:, h * 64:(h + 1) * 64], in0=pov[:, hh, 0:64], scalar1=rec[:, h:h + 1], scalar2=None,
                            op0=ALU.mult)), reads=[pok, "rec%d" % half], writes=[(yk, h)])
            ptrk = "pstr%d" % (g % 2)
            ptr_bf = pstr[:, (g % 2) * 512:(g % 2) * 512 + 512]
            for cc in range(4):
                P.op("pe", (lambda e, cc=cc, yatt=yatt, ptr_bf=ptr_bf: e.transpose(
                    out=ptr_bf[:, cc * 128:(cc + 1) * 128], in_=yatt[:, cc * 128:(cc + 1) * 128], identity=ident_bf[:, :])),
                    reads=[(yk, 2 * cc), (yk, 2 * cc + 1), "ident_bf"], writes=[ptrk])
            P.op("act" if g % 2 else "dve", (lambda e, g=g, ptr_bf=ptr_bf: (e.activation(
                out=yT[:, 4:8, g * 128:(g + 1) * 128], in_=ptr_bf[:, 0:512].rearrange("p (c t) -> p c t", c=4), func=AF.Copy)
                if g % 2 else e.tensor_copy(out=yT[:, 4:8, g * 128:(g + 1) * 128],
                                            in_=ptr_bf[:, 0:512].rearrange("p (c t) -> p c t", c=4)))),
                reads=[ptrk], writes=[("yTa", g)])
        P.barrier()

        if STAGE == 4:
            P.restore(snap0)
            P.op("dve", lambda e: e.memset(G1[:, :], 0.5), writes=["G1"])
            P.op("dve", lambda e: e.memset(A2[:, :], 1.0), writes=["A2"])
            P.op("dve", lambda e: e.memset(B2[:, :], 0.1), writes=["B2"])
            P.op("dve", lambda e: e.memset(arenaA[:, 24576:40960], 0.0), writes=["yTz"])
            P.barrier()
        if STAGE == 3:
            P.restore(snapA)
            P.op("dve", lambda e: e.memset(arenaA[:, 24576:40960], 0.0), writes=["yTz"])
            P.barrier()
        wo_bf = arenaA[:, 0:8192].rearrange("p (c n) -> p c n", c=8)
        for dc in range(8):
            st = stgs[dc % 2]
            key = "stg%d" % (dc % 2)
            P.dma(sp, (lambda e, st=st, dc=dc: e.dma_start(out=st[:, 0:1024], in_=w_out[dc * 128:(dc + 1) * 128, :])), writes=[key])
            P.op("pool" if dc % 2 else "dve", (lambda e, st=st, dc=dc: e.tensor_copy(out=wo_bf[:, dc, :], in_=st[:, 0:1024])),
                 reads=[key], writes=[("wo_bf", dc)])
        wokeys = [("wo_bf", dc) for dc in range(8)]
        h2T = arenaB[:, 0:16384].rearrange("p (c t) -> p c t", c=8)
        acc = arenaB_f[:, 8192:16384].rearrange("p (t d) -> p t d", t=8)
        stgG = arenaB_f[:, 16384:18432].rearrange("p (c f) -> p c f", c=8)
        stgU = arenaB_f[:, 18432:20480].rearrange("p (c f) -> p c f", c=8)
        h2f = stg1[:, 0:1024]
        h2hi = arenaA[:, 8192:9216]
        h2lo = arenaA[:, 9216:10240]
        wr_hi = arenaA[:, 10240:10368].rearrange("p (c e) -> p c e", c=8)
        wr_lo = arenaA[:, 10368:10496].rearrange("p (c e) -> p c e", c=8)
        h2Tlo = stg0[:, :].bitcast(BF16)[:, 2048:3072]
        hidT = arenaA[:, 0:16384].rearrange("p (c t) -> p c t", c=16)
        wd_bf = arenaA[:, 16384:32768].rearrange("p (c n) -> p c n", c=16)
        wg_bf = arenaA[:, 32768:36864].rearrange("p (b c f) -> p b c f", b=2, c=8)
        wu_bf = arenaA[:, 36864:40960].rearrange("p (b c f) -> p b c f", b=2, c=8)
        affE2 = arenaA_f[0:16, 0:8192]
        cmp2 = arenaA_f[0:16, 8192:16384]
        affE = arenaA_f[:, 0:2048].rearrange("p (q f) -> p q f", q=32)
        cmpb = arenaA_f[:, 2048:4096].rearrange("p (q f) -> p q f", q=32)
        affN = arenaA_f[:, 4096:6144].rearrange("p (b n) -> p b n", b=2)
        sgs = [stg0[:, 1024:1536], stg1[:, 1024:1536]]
        P.dma(sp, lambda e: e.dma_start(out=wr[:, :, :], in_=w_router.ap().rearrange("(c p) e -> p c e", p=128)), writes=["wr"])
        P.op("act", lambda e: e.activation(out=wr_hi, in_=wr[:, :, :], func=AF.Copy), reads=["wr"], writes=["wr_hi"])
        P.op("dve", lambda e: e.tensor_tensor(out=wr_lo, in0=wr[:, :, :], in1=wr_hi, op=ALU.subtract), reads=["wr", "wr_hi"], writes=["wr_lo"])
        for t in range(16):
            xt = xss[t % 2]
            xk = "xs%d" % (t % 2)
            P.dma(sp, (lambda e, xt=xt, t=t: e.dma_start(out=xt[:, :], in_=xh[ch, 256 + t * 128:256 + (t + 1) * 128, :])), writes=[xk])
            for half in range(2):
                pb = psb[(2 * t + half) % 4]
                pk = "ps%d" % ((2 * t + half) % 4)
                for kc in range(8):
                    P.op("pe", (lambda e, pb=pb, kc=kc, t=t, half=half: e.matmul(
                        pb[:, :], lhsT=yT[:, kc, t * 128:(t + 1) * 128], rhs=wo_bf[:, kc, half * 512:(half + 1) * 512],
                        start=(kc == 0), stop=(kc == 7))), reads=wokeys, writes=[pk])
                tmpf = stg0[:, half * 512:(half + 1) * 512]
                tk = "tmpf%d" % half
                P.op("dve", (lambda e, pb=pb, half=half, tmpf=tmpf: e.tensor_tensor(
                    out=tmpf, in0=pb[:, :], in1=G1[:, half * 512:(half + 1) * 512], op=ALU.mult)),
                    reads=[pk, "G1"], writes=[tk])
                P.op("pool", (lambda e, xt=xt, half=half, tmpf=tmpf: e.tensor_tensor(
                    out=xt[:, half * 512:(half + 1) * 512], in0=xt[:, half * 512:(half + 1) * 512], in1=tmpf, op=ALU.add)),
                    reads=[xk, tk], writes=[xk])
            P.dma(sp, (lambda e, xt=xt, t=t: e.dma_start(out=x1_d[t * 128:(t + 1) * 128, :], in_=xt[:, :])),
                  reads=[xk], writes=[("x1_d", t)])
            if STAGE == 1:
                P.dma(sp, (lambda e, xt=xt, t=t: e.dma_start(out=out_d[t * 128:(t + 1) * 128, :], in_=xt[:, :])),
                      reads=[xk], writes=[("out", t)])
                continue
            si = 2 + (t % 2)
            ssk = "ss%d" % si
            P.op("pool", (lambda e, si=si: e.memset(small[:, si:si + 1], 0.0)), writes=[ssk])
            P.op("act", (lambda e, xt=xt, si=si: e.activation(out=junk[:, :], in_=xt[:, :], func=AF.Square,
                                                              accum_out=small[:, si:si + 1])),
                 reads=[xk, ssk], writes=["junk", ssk])
            P.op("act", (lambda e, si=si: e.activation(out=small[:, si:si + 1], in_=small[:, si:si + 1], func=AF.Sqrt,
                                                       scale=1.0 / D, bias=EPS)), reads=[ssk], writes=[ssk])
            P.op("dve", (lambda e, si=si: e.reciprocal(out=small[:, si:si + 1], in_=small[:, si:si + 1])),
                 reads=[ssk], writes=[ssk])
            P.op("dve", (lambda e, xt=xt, si=si: e.scalar_tensor_tensor(out=h2f, in0=xt[:, :], scalar=small[:, si:si + 1],
                                                                        in1=A2[:, :], op0=ALU.mult, op1=ALU.mult)),
                 reads=[xk, ssk, "A2"], writes=["h2f"])
            P.op("pool", lambda e: e.tensor_tensor(out=h2f, in0=h2f, in1=B2[:, :], op=ALU.add), reads=["h2f", "B2"], writes=["h2f"])
            if SUB == 1:
                continue
            P.op("act", lambda e: e.activation(out=h2hi, in_=h2f, func=AF.Copy), reads=["h2f"], writes=["h2hi"])
            P.op("dve", lambda e: e.tensor_tensor(out=h2lo, in0=h2f, in1=h2hi, op=ALU.subtract), reads=["h2f", "h2hi"], writes=["h2lo"])
            for dc in range(8):
                P.op("pe", (lambda e, dc=dc: e.transpose(out=pstr[:, dc * 128:(dc + 1) * 128], in_=h2hi[:, dc * 128:(dc + 1) * 128],
                                                         identity=ident_bf[:, :])), reads=["h2hi", "ident_bf"], writes=["pstr"])
            P.op("act", (lambda e, t=t: e.activation(out=h2T[:, :, t * 128:(t + 1) * 128],
                                                     in_=pstr[:, :].rearrange("p (c t) -> p c t", c=8), func=AF.Copy)),
                 reads=["pstr"], writes=[("h2T", t)])
            for dc in range(8):
                P.op("pe", (lambda e, dc=dc: e.transpose(out=pstr[:, dc * 128:(dc + 1) * 128], in_=h2lo[:, dc * 128:(dc + 1) * 128],
                                                         identity=ident_bf[:, :])), reads=["h2lo", "ident_bf"], writes=["pstr"])
            P.op("dve", lambda e: e.tensor_copy(out=h2Tlo, in_=pstr[:, :]), reads=["pstr"], writes=["h2Tlo"])
            if SUB in (2, 5, 6, 7):
                continue
            k = 0
            for dc in range(8):
                for (lh, rh, lk) in ((h2T[:, dc, t * 128:(t + 1) * 128], wr_hi[:, dc, :], ("h2T", t)),
                                     (h2Tlo[:, dc * 128:(dc + 1) * 128], wr_hi[:, dc, :], "h2Tlo"),
                                     (h2T[:, dc, t * 128:(t + 1) * 128], wr_lo[:, dc, :], ("h2T", t))):
                    P.op("pe", (lambda e, lh=lh, rh=rh, k=k: e.matmul(psb[6][:, 0:16], lhsT=lh, rhs=rh, start=(k == 0), stop=(k == 23))),
                         reads=[lk, "wr_hi", "wr_lo"], writes=["ps6"])
                    k += 1
            if SUB == 3:
                continue
            P.op("dve", lambda e: e.reduce_max(out=small[:, 4:5], in_=psb[6][:, 0:16], axis=AX.X), reads=["ps6"], writes=["mx"])
            P.op("dve", lambda e: e.tensor_scalar(out=small[:, 4:5], in0=small[:, 4:5], scalar1=-1.0, scalar2=None, op0=ALU.mult),
                 reads=["mx"], writes=["mx"])
            P.op("pool", lambda e: e.memset(small[:, 5:6], 0.0), writes=["sm"])
            P.op("act", (lambda e, t=t: e.activation(out=aff_tm[:, t, :], in_=psb[6][:, 0:16], func=AF.Exp, bias=small[:, 4:5],
                                                     accum_out=small[:, 5:6])), reads=["ps6", "mx", "sm"], writes=[("aff", t), "sm"])
            P.op("dve", lambda e: e.reciprocal(out=small[:, 5:6], in_=small[:, 5:6]), reads=["sm"], writes=["sm"])
            P.op("dve", (lambda e, t=t: e.tensor_scalar(out=aff_tm[:, t, :], in0=aff_tm[:, t, :], scalar1=small[:, 5:6], scalar2=None,
                                                        op0=ALU.mult)), reads=[("aff", t), "sm"], writes=[("aff", t)])
        if STAGE != 1:
            for t in range(16):
                P.dma(sp, (lambda e, t=t, ch=ch: e.dma_start(out=aff_loc[ch, :, t * 128:(t + 1) * 128].rearrange("e p -> p e"),
                                                             in_=aff_tm[:, t, :], allow_slow_non_contiguous=True)),
                      reads=[("aff", t)], writes=["aff_loc"])
        P.barrier()
    def dbg_out():
        for t in range(16):
            P.dma(sp, (lambda e, t=t: e.dma_start(out=out_d[t * 128:(t + 1) * 128, 0:16], in_=w_tm[:, t, :])),
                  writes=[("out", t)])
            P.dma(sp, (lambda e, t=t: e.dma_start(out=out_d[t * 128:(t + 1) * 128, 16:32], in_=aff_tm[:, t, :])),
                  writes=[("out2", t)])
        P.dma(sp, lambda e: e.dma_start(out=out_d[0:128, 32:64], in_=bis[:, 0, :]), writes=["out3"])
        P.barrier()
        P.emit()

    if STAGE != 1:
        for c4 in range(NCH):
            P.dma(sp, (lambda e, c4=c4: e.dma_start(out=affE2[0:16, c4 * 2048:(c4 + 1) * 2048], in_=aff_loc[c4, :, :])),
                  reads=["aff_loc"], writes=["affE2"])
        bc(G2, mod_d[0:1, 5 * D:6 * D], "G2")
        bc(GF, g_fin[0:1, :], "GF")
        midc, gec, cntc, loc = bcol[0:16, 0:1], bcol[0:16, 1:2], bcol[0:16, 2:3], bcol[0:16, 3:4]
        P.op("dve", lambda e: e.memset(bcol[:, :], 0.5), writes=["midc"])
        for itn in range(NITER):
            ck = 2.0 ** -(itn + 1)
            P.op("dve", lambda e: e.tensor_scalar(out=cmp2, in0=affE2, scalar1=midc, scalar2=None, op0=ALU.is_ge),
                 reads=["affE2", "midc"], writes=["cmp2"])
            P.op("dve", lambda e: e.reduce_sum(out=cntc, in_=cmp2, axis=AX.X), reads=["cmp2"], writes=["cntc"])
            P.op("dve", lambda e: e.tensor_scalar(out=gec, in0=cntc, scalar1=1023.5, scalar2=-0.5, op0=ALU.is_ge, op1=ALU.add),
                 reads=["cntc"], writes=["gec"])
            P.op("dve", (lambda e, ck=ck: e.scalar_tensor_tensor(out=midc, in0=gec, scalar=ck, in1=midc, op0=ALU.mult, op1=ALU.add)),
                 reads=["gec", "midc"], writes=["midc"])
        P.op("dve", lambda e: e.tensor_scalar(out=loc, in0=midc, scalar1=-(2.0 ** -(NITER + 1)), scalar2=None, op0=ALU.add),
             reads=["midc"], writes=["loc"])
        P.dma(sp, lambda e: e.dma_start(out=thr_d.ap().rearrange("o q -> q o"), in_=loc, allow_slow_non_contiguous=True),
              reads=["loc"], writes=["thr_d"])
        bt = bis
        lo_ = bt[:, 0, 0:16]
        P.dma(sp, lambda e: e.dma_start(out=lo_, in_=thr_d[0:1, :].partition_broadcast(128)), reads=["thr_d"], writes=["lo"])
        thr = lo_
        for t in range(16):
            P.op("dve", (lambda e, t=t: e.tensor_tensor(out=w_tm[:, t, :], in0=aff_tm[:, t, :], in1=thr, op=ALU.is_ge)),
                 reads=[("aff", t), "lo"], writes=[("w", t)])
            P.op("pool", (lambda e, t=t: e.tensor_tensor(out=w_tm[:, t, :], in0=w_tm[:, t, :], in1=aff_tm[:, t, :], op=ALU.mult)),
                 reads=[("aff", t), ("w", t)], writes=[("w", t)])
        P.barrier()
        if STAGE in (2, 3, 4):
            for t in range(16):
                P.dma(sp, (lambda e, t=t: e.dma_start(out=out_d[t * 128:(t + 1) * 128, 0:16], in_=w_tm[:, t, :])),
                      writes=[("out", t)])
                P.dma(sp, (lambda e, t=t: e.dma_start(out=out_d[t * 128:(t + 1) * 128, 16:32], in_=aff_tm[:, t, :])),
                      writes=[("out2", t)])
            P.dma(sp, lambda e: e.dma_start(out=out_d[0:128, 32:64], in_=bis[:, 0, :]), writes=["out3"])
            P.barrier()
            P.emit()
            return nc
        wgv = w_gate.ap().rearrange("e (c p) f -> e p c f", p=128)
        wuv = w_up.ap().rearrange("e (c p) f -> e p c f", p=128)
        step = 0
        for th in range(2):
            P.op("pool", lambda e: e.memset(acc[:, :, :], 0.0), writes=[("acc", i) for i in range(8)])
            for ex in range(NEXP):
                for fb in range(8):
                    bi = step % 2
                    step += 1
                    for (wv, stg_, wbf, nm, ceng) in ((wgv, stgG, wg_bf, "g", "dve"), (wuv, stgU, wu_bf, "u", "pool")):
                        P.dma(sp, (lambda e, wv=wv, stg_=stg_, ex=ex, fb=fb: e.dma_start(
                            out=stg_, in_=wv[ex, :, :, fb * 256:(fb + 1) * 256])), writes=["stg" + nm])
                        P.op(ceng, (lambda e, stg_=stg_, wbf=wbf, bi=bi: e.tensor_copy(out=wbf[:, bi, :, :], in_=stg_)),
                             reads=["stg" + nm], writes=[("w" + nm, bi)])
                    for k2 in range(2):
                        fc = fb * 2 + k2
                        st = stgs[k2]
                        sk = "stg%d" % k2
                        P.dma(sp, (lambda e, st=st, ex=ex, fc=fc: e.dma_start(out=st[:, 0:1024],
                                                                             in_=w_down[ex, fc * 128:(fc + 1) * 128, :])),
                              writes=[sk])
                        P.op("pool" if k2 else "act", (lambda e, st=st, fc=fc, k2=k2: (
                            e.tensor_copy(out=wd_bf[:, fc, :], in_=st[:, 0:1024]) if k2 else
                            e.activation(out=wd_bf[:, fc, :], in_=st[:, 0:1024], func=AF.Copy))),
                            reads=[sk], writes=[("wd", fc)])
                    for k2 in range(2):
                        fc = fb * 2 + k2
                        for nb in range(2):
                            pg_, pu_ = psb[(2 * (k2 * 2 + nb)) % 4], psb[(2 * (k2 * 2 + nb)) % 4 + 1]
                            kg_, ku_ = "ps%d" % ((2 * (k2 * 2 + nb)) % 4), "ps%d" % ((2 * (k2 * 2 + nb)) % 4 + 1)
                            tok0 = th * 1024 + nb * 512
                            for (pp, kk, wbf, nm) in ((pg_, kg_, wg_bf, "g"), (pu_, ku_, wu_bf, "u")):
                                for dc in range(8):
                                    P.op("pe", (lambda e, pp=pp, wbf=wbf, bi=bi, dc=dc, k2=k2, tok0=tok0: e.matmul(
                                        pp[:, :], lhsT=wbf[:, bi, dc, k2 * 128:(k2 + 1) * 128], rhs=h2T[:, dc, tok0:tok0 + 512],
                                        start=(dc == 0), stop=(dc == 7))), reads=[("w" + nm, bi)], writes=[kk])
                            sgt = sgs[nb]
                            sgk = "sg%d" % nb
                            P.op("act", (lambda e, pg_=pg_, sgt=sgt: e.activation(out=sgt, in_=pg_[:, :], func=AF.Silu)),
                                 reads=[kg_], writes=[sgk])
                            P.op("dve", (lambda e, pu_=pu_, sgt=sgt, fc=fc, nb=nb: e.tensor_tensor(
                                out=hidT[:, fc, nb * 512:(nb + 1) * 512], in0=pu_[:, :], in1=sgt, op=ALU.mult)),
                                reads=[ku_, sgk], writes=[("hid", fc)])
                for tl in range(8):
                    for half in range(2):
                        pb = psb[4 + (2 * tl + half) % 3]
                        pk = "ps%d" % (4 + (2 * tl + half) % 3)
                        for fc in range(16):
                            P.op("pe", (lambda e, pb=pb, fc=fc, tl=tl, half=half: e.matmul(
                                pb[:, :], lhsT=hidT[:, fc, tl * 128:(tl + 1) * 128], rhs=wd_bf[:, fc, half * 512:(half + 1) * 512],
                                start=(fc == 0), stop=(fc == 15))), reads=[("hid", fc), ("wd", fc)], writes=[pk])
                        P.op("dve", (lambda e, pb=pb, tl=tl, half=half, th=th, ex=ex: e.scalar_tensor_tensor(
                            out=acc[:, tl, half * 512:(half + 1) * 512], in0=pb[:, :], scalar=w_tm[:, th * 8 + tl, ex:ex + 1],
                            in1=acc[:, tl, half * 512:(half + 1) * 512], op0=ALU.mult, op1=ALU.add)),
                            reads=[pk, ("acc", tl)], writes=[("acc", tl)])
            for tl in range(8):
                t = th * 8 + tl
                xt = xss[t % 2]
                xk = "xs%d" % (t % 2)
                si = 6 + (t % 2)
                ssk = "ss%d" % si
                P.dma(sp, (lambda e, xt=xt, t=t: e.dma_start(out=xt[:, :], in_=x1_d[t * 128:(t + 1) * 128, :])), writes=[xk])
                P.op("dve", (lambda e, tl=tl: e.tensor_tensor(out=acc[:, tl, :], in0=acc[:, tl, :], in1=G2[:, :], op=ALU.mult)),
                     reads=[("acc", tl), "G2"], writes=[("acc", tl)])
                P.op("pool", (lambda e, xt=xt, tl=tl: e.tensor_tensor(out=xt[:, :], in0=xt[:, :], in1=acc[:, tl, :], op=ALU.add)),
                     reads=[xk, ("acc", tl)], writes=[xk])
                P.op("pool", (lambda e, si=si: e.memset(small[:, si:si + 1], 0.0)), writes=[ssk])
                P.op("act", (lambda e, xt=xt, si=si: e.activation(out=junk[:, :], in_=xt[:, :], func=AF.Square,
                                                                  accum_out=small[:, si:si + 1])),
                     reads=[xk, ssk], writes=["junk", ssk])
                P.op("act", (lambda e, si=si: e.activation(out=small[:, si:si + 1], in_=small[:, si:si + 1], func=AF.Sqrt,
                                                           scale=1.0 / D, bias=EPS)), reads=[ssk], writes=[ssk])
                P.op("dve", (lambda e, si=si: e.reciprocal(out=small[:, si:si + 1], in_=small[:, si:si + 1])),
                     reads=[ssk], writes=[ssk])
                P.op("dve", (lambda e, xt=xt, si=si: e.scalar_tensor_tensor(out=xt[:, :], in0=xt[:, :], scalar=small[:, si:si + 1],
                                                                            in1=GF[:, :], op0=ALU.mult, op1=ALU.mult)),
                     reads=[xk, ssk, "GF"], writes=[xk])
                P.dma(sp, (lambda e, xt=xt, t=t: e.dma_start(out=out_d[t * 128:(t + 1) * 128, :], in_=xt[:, :])),
                      reads=[xk], writes=[("out", t)])
    P.barrier()
    P.emit()
    return nc


def _bias_tables(rpb, j):
    row0 = 32 * j
    p = np.arange(128)
    a, kc = p // 64, p % 64
    qq = np.arange(128)
    b_, qc = qq // 64, qq % 64
    c0 = np.clip(qc - 8, 0, 48)
    colv = (kc[:, None] >= c0[None, :]) & (kc[:, None] < c0[None, :] + 16)
    dcol = np.clip(kc[:, None] - qc[None, :] + 15, 0, 30)
    tab = np.full((16, 128, 8, 6, 128), NEG, np.float32)
    for g in range(16):
        lo = min(max(2 * g - 2, 0), 28)
        r = row0 + 2 * g + b_
        r0 = np.clip(r - 4, 0, 120)
        for jt in range(6):
            gkr = row0 + lo + 2 * jt + a - 4
            rowv = (gkr[:, None] >= 0) & (gkr[:, None] < 128) & (gkr[:, None] >= r0[None, :]) & (gkr[:, None] < r0[None, :] + 8)
            valid = rowv & colv
            drow = np.clip(gkr[:, None] - r[None, :] + 7, 0, 14)
            vals = rpb[:, drow, dcol]
            tab[g, :, :, jt, :] = np.where(valid[:, None, :], np.transpose(vals, (1, 0, 2)), np.float32(NEG))
    return tab.reshape(16, 128, 8 * 6 * 128)


_NC_CACHE = {}


def kernel(x, c, ctx, c_ctx, w_mod, b_mod, norm_mix_g, w_in, conv_w, conv_b, conv_ln_g, conv_ln_b,
           rpb, w_out, norm_ffn_g, w_router, w_gate, w_up, w_down, norm_final_g):
    f = lambda a: np.ascontiguousarray(np.asarray(a, dtype=np.float32))
    x, c, ctx, c_ctx = f(x), f(c), f(ctx), f(c_ctx)
    if "nc" not in _NC_CACHE:
        _NC_CACHE["nc"] = build_nc()
    nc = _NC_CACHE["nc"]
    ident = np.eye(128, dtype=np.float32)
    wg_full, wu_full, wd_full = f(w_gate[0][:NEXP]), f(w_up[0][:NEXP]), f(w_down[0][:NEXP])
    in_maps = []
    rpb0 = f(rpb[0])
    tabs = [_bias_tables(rpb0, jj) for jj in range(4)]
    base_tab = np.ascontiguousarray(tabs[1][[0, 1, 14, 15, 2]])
    patch_tab = np.ascontiguousarray(np.stack([tabs[0][0], tabs[0][1], tabs[3][14], tabs[3][15]]))
    for i in range(NCORE):
        b, j = i // 4, i % 4
        order = [jj for jj in range(4) if jj != j] + [j]
        xh = np.zeros((NCH, HALO, D), np.float32)
        tokmask = np.zeros((NCH, 1, HALO), np.float32)
        bsel = np.zeros((128, NCH, 2), np.float32)
        for c4, jj in enumerate(order):
            start = 2048 * jj - 256
            lo, hi = max(start, 0), min(start + HALO, T)
            xh[c4, lo - start:hi - start] = x[b, lo:hi]
            tokmask[c4, 0, lo - start:hi - start] = 1.0
            bsel[:, c4, 0] = 1.0 if jj == 0 else 0.0
            bsel[:, c4, 1] = 1.0 if jj == 3 else 0.0
        in_maps.append({
            "xh": xh, "tokmask": tokmask, "cvec": np.stack([c[b], c_ctx]), "ctx": f(ctx[b]),
            "w_mod": f(w_mod[0]), "b_mod": f(b_mod[0])[None, :], "norm_mix_g": f(norm_mix_g[0])[None, :],
            "norm_ffn_g": f(norm_ffn_g[0])[None, :], "norm_final_g": f(norm_final_g)[None, :],
            "w_in": f(w_in[0]), "conv_w": f(conv_w[0]), "conv_b": f(conv_b[0])[None, :],
            "conv_ln_g": f(conv_ln_g[0])[None, :], "conv_ln_b": f(conv_ln_b[0])[None, :],
            "bias_tab": base_tab, "bias_patch": patch_tab, "bsel": bsel, "w_out": f(w_out[0]), "w_router": f(w_router[0]),
            "w_gate": wg_full, "w_up": wu_full, "w_down": wd_full,
            "ident": ident,
        })
    if STAGE == 4:
        tiny = {"w_mod": [128, 8], "w_in": [128, 8], "bias_tab": [16, 1, 8], "w_gate": [1, 128, 8],
                "w_up": [1, 128, 8], "w_down": [1, 128, 8]}
        for m in in_maps:
            for k, shp in tiny.items():
                m[k] = np.zeros(shp, np.float32)
    if _NC_CACHE.get("prep_only"):
        return in_maps
    res = run_bass_kernel_spmd(nc, in_maps, core_ids=list(range(NCORE)))
    out = np.concatenate([res.results[i]["out"] for i in range(NCORE)], axis=0)
    return out.reshape(2, T, D).astype(np.float32)
```
